# Optimizing a Trainium2 kernel written in Bass

```python
import math
import jax, jax.numpy as jnp
from jax import lax
import numpy as np

D_MODEL = 1024
BATCH = 16
SEQ = 4096
DEPTH = 1

CHUNK = 64
LEFT_CHUNKS = 8
A_HEADS = 8
A_HEAD_DIM = 64
A_WIDTH = A_HEADS * A_HEAD_DIM
REL_CLIP = 128
B_HEADS = 4
B_HEAD_DIM = 128
B_WIDTH = B_HEADS * B_HEAD_DIM
CONV_K = 4
PEER_HEADS = 8
N_KEYS = 128
N_EXPERTS = N_KEYS * N_KEYS
PEER_TOPK = 16
D_KEY = 256
PEER_BLOCK = 128
EPS = 1e-6
IN_SIZES = [A_WIDTH, A_WIDTH, A_WIDTH, 2 * B_WIDTH, B_WIDTH, B_WIDTH, B_HEADS, B_HEADS, D_MODEL, D_MODEL]
IN_COLS = sum(IN_SIZES)

kernel_name = "hybrid_chunk_attn_mlstm_peer_block"


def rmsnorm(x, g):
    xf = x.astype(jnp.float32)
    y = xf * lax.rsqrt(jnp.mean(xf * xf, axis=-1, keepdims=True) + EPS)
    return (y * g.astype(jnp.float32)).astype(x.dtype)


def modulate(h, shift, scale):
    return h * (1.0 + scale[:, None, :]) + shift[:, None, :]


def causal_depthwise_conv(u, w, b):
    C = u.shape[-1]
    out = lax.conv_general_dilated(
        u, w[:, None, :].astype(u.dtype), window_strides=(1,), padding=[(CONV_K - 1, 0)],
        dimension_numbers=("NWC", "WIO", "NWC"), feature_group_count=C)
    return out + b


def chunk_band_attention(q, k, v, rel_bias):
    B, S, H, dh = q.shape
    nc = S // CHUNK
    pad = LEFT_CHUNKS * CHUNK
    band = pad + CHUNK
    kp = jnp.pad(k, ((0, 0), (pad, 0), (0, 0), (0, 0)))
    vp = jnp.pad(v, ((0, 0), (pad, 0), (0, 0), (0, 0)))
    qc = q.reshape(B, nc, CHUNK, H, dh).transpose(1, 0, 2, 3, 4)
    k_idx = jnp.arange(band)
    rel = jnp.clip(jnp.arange(CHUNK)[:, None] + pad - k_idx[None, :], -REL_CLIP, REL_CLIP) + REL_CLIP
    bias = rel_bias[:, rel].astype(jnp.float32)
    scale = dh ** -0.5

    def one_chunk(args):
        j, qj = args
        kj = lax.dynamic_slice_in_dim(kp, j * CHUNK, band, axis=1)
        vj = lax.dynamic_slice_in_dim(vp, j * CHUNK, band, axis=1)
        s = jnp.einsum("blhd,bkhd->bhlk", qj, kj).astype(jnp.float32) * scale + bias
        valid = (j * CHUNK + k_idx - pad) >= 0
        s = jnp.where(valid[None, None, None, :], s, -jnp.inf)
        p = jax.nn.softmax(s, axis=-1).astype(vj.dtype)
        return jnp.einsum("bhlk,bkhd->blhd", p, vj)

    out = lax.map(one_chunk, (jnp.arange(nc), qc))
    return out.transpose(1, 0, 2, 3, 4).reshape(B, S, H * dh)


def mlstm_chunkwise(q, k, v, i_pre, f_pre):
    B, S, H, d = q.shape
    nc = S // CHUNK
    k = k * (d ** -0.5)
    to_c = lambda t: t.reshape(B, nc, CHUNK, H, d).transpose(1, 0, 3, 2, 4)
    qc, kc, vc = to_c(q), to_c(k), to_c(v)
    li = i_pre.astype(jnp.float32).reshape(B, nc, CHUNK, H).transpose(1, 0, 3, 2)
    lf = jax.nn.log_sigmoid(f_pre.astype(jnp.float32)).reshape(B, nc, CHUNK, H).transpose(1, 0, 3, 2)
    a = jnp.cumsum(lf, axis=-1)
    A = a[..., -1]
    causal = jnp.tril(jnp.ones((CHUNK, CHUNK), dtype=bool))

    def step(carry, inp):
        C, n, m = carry
        qj, kj, vj, aj, Aj, lij = inp
        D = aj[..., :, None] - aj[..., None, :] + lij[..., None, :]
        D = jnp.where(causal, D, -jnp.inf)
        inter = aj + m[..., None]
        m_row = jnp.maximum(inter, jnp.max(D, axis=-1))
        w_inter = jnp.exp(inter - m_row)
        Wd = jnp.exp(D - m_row[..., None])
        qk = jnp.einsum("bhld,bhsd->bhls", qj, kj).astype(jnp.float32) * Wd
        num = w_inter[..., None] * jnp.einsum("bhld,bhde->bhle", qj, C) + jnp.einsum("bhls,bhse->bhle", qk, vj)
        den = w_inter * jnp.einsum("bhld,bhd->bhl", qj, n) + jnp.sum(qk, axis=-1)
        h = num / jnp.maximum(jnp.abs(den), jnp.exp(-m_row))[..., None]
        g = Aj[..., None] - aj + lij
        m_new = jnp.maximum(Aj + m, jnp.max(g, axis=-1))
        decay = jnp.exp(Aj + m - m_new)
        wk = jnp.exp(g - m_new[..., None])
        C_new = decay[..., None, None] * C + jnp.einsum("bhl,bhld,bhle->bhde", wk, kj, vj)
        n_new = decay[..., None] * n + jnp.einsum("bhl,bhld->bhd", wk, kj)
        return (C_new, n_new, m_new), h

    init = (jnp.zeros((B, H, d, d), jnp.float32), jnp.zeros((B, H, d), jnp.float32), jnp.zeros((B, H), jnp.float32))
    _, hs = lax.scan(step, init, (qc, kc, vc, a, A, li))
    return hs.transpose(1, 0, 3, 2, 4).reshape(B, S, H, d).astype(v.dtype)


def peer_ffn(h, w_pq, sub_keys, u_exp, v_exp):
    B, S, D = h.shape
    T = B * S
    hb = h.reshape(T // PEER_BLOCK, PEER_BLOCK, D)

    def block(xb):
        q = (xb @ w_pq).reshape(PEER_BLOCK, PEER_HEADS, 2, D_KEY // 2)
        s = jnp.einsum("thpd,hpnd->thpn", q, sub_keys).astype(jnp.float32)
        s_top, i_top = lax.top_k(s, PEER_TOPK)
        cand = (s_top[:, :, 0, :, None] + s_top[:, :, 1, None, :]).reshape(PEER_BLOCK, PEER_HEADS, PEER_TOPK * PEER_TOPK)
        best, ci = lax.top_k(cand, PEER_TOPK)
        i1 = jnp.take_along_axis(i_top[:, :, 0, :], ci // PEER_TOPK, axis=-1)
        i2 = jnp.take_along_axis(i_top[:, :, 1, :], ci % PEER_TOPK, axis=-1)
        e = i1 * N_KEYS + i2
        gate = jax.nn.softmax(best, axis=-1)
        u = u_exp[e]
        v = v_exp[e]
        act = jax.nn.gelu(jnp.einsum("thkd,td->thk", u, xb))
        return jnp.einsum("thk,thkd->td", (gate * act).astype(xb.dtype), v)

    return lax.map(block, hb).reshape(B, S, D)


def setup_inputs(seed: int = 0) -> dict:
    key = jax.random.key(seed)
    ks = jax.random.split(key, 24)
    nrm = lambda k, shape, s: jax.random.normal(k, shape, jnp.float32) * s
    L = DEPTH
    return {
        "x": nrm(ks[0], (BATCH, SEQ, D_MODEL), 1.0),
        "c": nrm(ks[1], (BATCH, D_MODEL), 1.0),
        "w_ada": nrm(ks[2], (L, D_MODEL, 6 * D_MODEL), 0.5 * D_MODEL ** -0.5),
        "b_ada": nrm(ks[3], (L, 6 * D_MODEL), 0.02),
        "norm1_g": 1.0 + nrm(ks[4], (L, D_MODEL), 0.02),
        "w_in": nrm(ks[5], (L, D_MODEL, IN_COLS), D_MODEL ** -0.5),
        "conv_w": nrm(ks[6], (L, CONV_K, 2 * B_WIDTH), CONV_K ** -0.5),
        "conv_b": nrm(ks[7], (L, 2 * B_WIDTH), 0.02),
        "b_igate": nrm(ks[8], (L, B_HEADS), 0.1),
        "b_fgate": jnp.linspace(3.0, 6.0, B_HEADS)[None, :] + nrm(ks[9], (L, B_HEADS), 0.1),
        "rel_bias": nrm(ks[10], (L, A_HEADS, 2 * REL_CLIP + 1), 0.1),
        "mlstm_norm_g": 1.0 + nrm(ks[11], (L, B_WIDTH), 0.02),
        "w_branch_a": nrm(ks[12], (L, A_WIDTH, D_MODEL), A_WIDTH ** -0.5),
        "w_branch_b": nrm(ks[13], (L, B_WIDTH, D_MODEL), B_WIDTH ** -0.5),
        "w_out": nrm(ks[14], (L, D_MODEL, D_MODEL), D_MODEL ** -0.5),
        "norm2_g": 1.0 + nrm(ks[15], (L, D_MODEL), 0.02),
        "w_peer_q": nrm(ks[16], (L, D_MODEL, PEER_HEADS * D_KEY), D_MODEL ** -0.5),
        "peer_sub_keys": nrm(ks[17], (L, PEER_HEADS, 2, N_KEYS, D_KEY // 2), (D_KEY // 2) ** -0.5),
        "peer_u": nrm(ks[18], (L, N_EXPERTS, D_MODEL), D_MODEL ** -0.5),
        "peer_v": nrm(ks[19], (L, N_EXPERTS, D_MODEL), 0.5),
        "final_g": 1.0 + nrm(ks[20], (D_MODEL,), 0.02),
    }


def reference(x, c, w_ada, b_ada, norm1_g, w_in, conv_w, conv_b, b_igate, b_fgate, rel_bias, mlstm_norm_g,
              w_branch_a, w_branch_b, w_out, norm2_g, w_peer_q, peer_sub_keys, peer_u, peer_v, final_g):
    B, S, _ = x.shape
    split_at = [int(s) for s in np.cumsum(IN_SIZES)[:-1]]
    for l in range(DEPTH):
        mod = jax.nn.silu(c) @ w_ada[l] + b_ada[l]
        sh1, sc1, gt1, sh2, sc2, gt2 = jnp.split(mod, 6, axis=-1)

        h = modulate(rmsnorm(x, norm1_g[l]), sh1, sc1)
        proj = h @ w_in[l]
        qa, ka, va, qkb, vb, ob, ib, fb, ga, gb = jnp.split(proj, split_at, axis=-1)
        qkb = jax.nn.silu(causal_depthwise_conv(qkb, conv_w[l], conv_b[l]))
        qb, kb = jnp.split(qkb, 2, axis=-1)
        heads_a = lambda t: t.reshape(B, S, A_HEADS, A_HEAD_DIM)
        heads_b = lambda t: t.reshape(B, S, B_HEADS, B_HEAD_DIM)
        y_a = chunk_band_attention(heads_a(qa), heads_a(ka), heads_a(va), rel_bias[l])
        hb = mlstm_chunkwise(heads_b(qb), heads_b(kb), heads_b(vb), ib + b_igate[l], fb + b_fgate[l])
        hb = rmsnorm(hb, mlstm_norm_g[l].reshape(B_HEADS, B_HEAD_DIM)).reshape(B, S, B_WIDTH)
        y_b = jax.nn.sigmoid(ob) * hb
        y = jax.nn.sigmoid(ga) * (y_a @ w_branch_a[l]) + jax.nn.sigmoid(gb) * (y_b @ w_branch_b[l])
        x = x + gt1[:, None, :] * (y @ w_out[l])

        h2 = modulate(rmsnorm(x, norm2_g[l]), sh2, sc2)
        x = x + gt2[:, None, :] * peer_ffn(h2, w_peer_q[l], peer_sub_keys[l], peer_u[l], peer_v[l])
    return rmsnorm(x, final_g)
```

```python
import numpy as np
from contextlib import ExitStack
import concourse.bass as bass
import concourse.mybir as mybir
from concourse.bass_utils import run_bass_kernel_spmd

F32 = mybir.dt.float32
BF16 = mybir.dt.bfloat16
U32 = mybir.dt.uint32
I32 = mybir.dt.int32
ALU = mybir.AluOpType
AF = mybir.ActivationFunctionType
AX = mybir.AxisListType

D = 1024
IN_COLS = 5640
EPS = 1e-6
NEG = -1.0e30
ENGS = ("pe", "act", "dve", "pool", "sp")


class Res:
    __slots__ = ("name", "w", "r", "excl")

    def __init__(self, name, excl=False):
        self.name = name
        self.w = None
        self.r = []
        self.excl = excl


class Chan:
    def __init__(self, sem, name):
        self.sem = sem
        self.name = name
        self.n = 0
        self.last = None


class _Rec:
    def __getattr__(self, name):
        return lambda *a, **k: (name, a, k)


_REC = _Rec()


class Sched:
    def __init__(self, nc, es):
        self.nc = nc
        self.es = es
        self.ops = []
        self.chans = []
        self.sems = {e: es.enter_context(nc.semaphore("eng_" + e)) for e in ENGS}
        self.emitted = 0
        self.cnt = {e: 0 for e in ENGS}
        self.known = {e: {} for e in ENGS}
        self.prog = {}
        self.clock = {}

    def chan(self, name):
        c = Chan(self.es.enter_context(self.nc.semaphore("ch_" + name)), name)
        self.chans.append(c)
        return c

    limit = None
    in_bar = False

    def op(self, eng, fn, reads=(), writes=(), chan=None, extra=()):
        oid = len(self.ops)
        if self.limit is not None and oid >= self.limit and not self.in_bar:
            return None
        deps = set(extra)
        for r in reads:
            if r.w is not None:
                deps.add(r.w)
            if r.excl:
                deps.update(r.r)
        for w in writes:
            if w.w is not None:
                deps.add(w.w)
            deps.update(w.r)
        if chan is not None and chan.last is not None:
            deps.add(chan.last)
        for r in reads:
            if r.excl:
                r.w = oid
                r.r = []
            else:
                r.r.append(oid)
        for w in writes:
            w.w = oid
            w.r = []
        if chan is not None:
            chan.last = oid
        deps.discard(oid)
        name, a, k = fn(_REC)
        fn = (lambda e, name=name, a=a, k=k: getattr(e, name)(*a, **k))
        self.ops.append((eng, fn, deps, chan))
        return oid

    def dma(self, eng, chan, out, in_, reads=(), writes=()):
        return self.op(eng, lambda e: e.dma_start(out=out, in_=in_), reads, writes, chan=chan)

    def barrier(self, bres):
        dum, dps, dd, cbar = self.bar_objs
        self.in_bar = True
        fns = {
            "pe": lambda g: g.matmul(dps[0:2, 0:2], dum[:, 0:2], dum[:, 2:4], start=True, stop=True,
                                     skip_group_check=True),
            "act": lambda g: g.activation(out=dum[:, 4:5], in_=dum[:, 0:1], func=AF.Copy),
            "dve": lambda g: g.tensor_copy(out=dum[:, 5:6], in_=dum[:, 0:1]),
            "pool": lambda g: g.tensor_copy(out=dum[:, 6:7], in_=dum[:, 0:1]),
        }
        for rnd in range(2):
            for e in ENGS:
                rd = ([bres[f] for f in ENGS] if rnd == 1 else []) + [bres["dum"]]
                if e == "sp":
                    ex = [c.last for c in self.chans if c.last is not None]
                    self.op("sp", lambda g, rnd=rnd: g.dma_start(out=dd[rnd:rnd + 1, :], in_=self.bar_src), reads=rd, writes=[bres[e]],
                            chan=cbar, extra=ex)
                else:
                    self.op(e, fns[e], reads=rd + ([bres["pe_bank"]] if e == "pe" else []), writes=[bres[e]])
        self.in_bar = False

    def emit(self):
        ops = self.ops
        lo = self.emitted
        hi = len(ops)
        needed = set()
        for i in range(lo, hi):
            eng, fn, deps, chan = ops[i]
            for d in deps:
                de, _, _, dc = ops[d]
                if de == "pe" and eng == "pe" and dc is None and chan is None:
                    continue
                if d >= lo:
                    needed.add(d)
        per_eng = {e: [] for e in ENGS}
        for i in range(lo, hi):
            eng, fn, deps, chan = ops[i]
            waits = []
            kn = self.known[eng]
            for d in sorted(deps):
                de, _, _, dc = ops[d]
                if de == "pe" and eng == "pe" and dc is None and chan is None:
                    continue
                if d < lo:
                    continue
                key, val = self.prog[d]
                if kn.get(key, 0) >= val:
                    continue
                waits.append((key, val))
                kn[key] = val
                ck = self.clock.get(d)
                if ck is not None:
                    for k2, v2 in ck.items():
                        if kn.get(k2, 0) < v2:
                            kn[k2] = v2
            wmax = {}
            for key, val in waits:
                if wmax.get(key, 0) < val:
                    wmax[key] = val
            inc = None
            if chan is not None:
                chan.n += 1
                self.prog[i] = (chan, chan.n)
                self.clock[i] = {k: v for k, v in kn.items() if isinstance(k, str)}
                inc = (chan.sem, 16)
            elif i in needed:
                self.cnt[eng] += 1
                self.prog[i] = (eng, self.cnt[eng])
                ck = {k: v for k, v in kn.items() if isinstance(k, str)}
                ck[eng] = self.cnt[eng]
                self.clock[i] = ck
                inc = (self.sems[eng], 1)
            per_eng[eng].append((list(wmax.items()), fn, inc))
        self.emitted = hi
        sems = self.sems

        def replay(e, lst):
            for waits, fn, inc in lst:
                for key, val in waits:
                    if isinstance(key, str):
                        e.wait_ge(sems[key], val)
                    else:
                        e.wait_ge(key.sem, val * 16)
                ins = fn(e)
                if inc is not None:
                    ins.then_inc(inc[0], inc[1])

        with self.nc.Block() as block:
            @block.tensor
            def _(e):
                replay(e, per_eng["pe"])

            @block.scalar
            def _(e):
                replay(e, per_eng["act"])

            @block.vector
            def _(e):
                replay(e, per_eng["dve"])

            @block.gpsimd
            def _(e):
                replay(e, per_eng["pool"])

            @block.sync
            def _(e):
                replay(e, per_eng["sp"])


def build(NB, S, debug=False, do_b=True):
    T = NB * S
    NT = S // 128
    nc = bass.Bass("TRN2", target_bir_lowering=False)

    def din(name, shape, dt=F32):
        return nc.dram_tensor(name, list(shape), dt, kind="ExternalInput").ap()

    x = din("x", [T, D])
    cT = din("cT", [128, NB, 8])
    w_ada = din("w_ada", [D, 6 * D])
    b_ada = din("b_ada", [1, 6 * D])
    g1 = din("norm1_g", [1, D])
    w_in = din("w_in", [D, IN_COLS])
    cwT = din("cwT", [128, 8, 4])
    cbT = din("cbT", [128, 8])
    bif = din("bif", [1, 8])
    ebsrc = din("ebsrc", [128, 4, 8, 128])
    gmn_d = din("gmn", [1, 512])
    wba_d = din("w_branch_a", [512, D])
    wbb_d = din("w_branch_b", [512, D])
    wout_d = din("w_out", [D, D])
    g2 = din("norm2_g", [1, D])
    wpq_d = din("w_peer_q", [D, 2048])
    keysT = din("keysT", [128, 16, 128])
    pu = din("peer_u", [16384, D])
    pv = din("peer_v", [16384, D])
    fg = din("final_g", [1, D])
    consts_d = din("consts", [128, 6, 128])
    iota_d = din("iota16", [128, 2048])
    out = nc.dram_tensor("out", [T, D], F32, kind="ExternalOutput").ap()
    modrow = nc.dram_tensor("modrow", [NB, 6 * D], F32, kind="Internal").ap()
    x1d = nc.dram_tensor("x1d", [T, D], F32, kind="Internal").ap()
    uvd = nc.dram_tensor("uvd", [16384, 2 * D], BF16, kind="Internal").ap()
    dbg = None
    if debug:
        dbg = nc.dram_tensor("dbg", [T, D], F32, kind="ExternalOutput").ap()

    es = ExitStack()
    with es:
        S_ = Sched(nc, es)
        op = S_.op
        bres = {e: Res("bar_" + e) for e in ENGS}
        dum = es.enter_context(nc.sbuf_tensor("bar_dum", [128, 8], F32))
        ddr = nc.dram_tensor("bar_dd", [2, 16], F32, kind="Internal").ap()
        ps = ExitStack()
        es.enter_context(ps)
        B = [ps.enter_context(nc.psum_tensor("pb%d" % i, [128, 512], F32)) for i in range(6)]
        Tb = [ps.enter_context(nc.psum_tensor("pt%d" % i, [128, 1024], BF16)) for i in range(2)]
        RB = [Res("B%d" % i, excl=True) for i in range(6)]
        RT = [Res("T%d" % i, excl=True) for i in range(2)]
        bres["pe_bank"] = RB[5]
        S_.bar_objs = (dum, B[5], ddr, S_.chan("bar"))
        S_.bar_src = b_ada[0:1, 0:16]
        bres["dum"] = Res("dum")
        op("pool", lambda e: e.memset(dum[:], 0.0), writes=[bres["dum"]])

        with ExitStack() as p0:
            def sb(name, shape, dt=F32):
                return p0.enter_context(nc.sbuf_tensor("p0_" + name, list(shape), dt))
            stg = [sb("p0stg%d" % i, [128, 1024]) for i in range(3)]
            Rstg = [Res("p0stg%d" % i) for i in range(3)]
            cstg = [S_.chan("p0stg%d" % i) for i in range(3)]
            scb = sb("scb", [128, NB, 8])
            Rscb = Res("scb")
            brow = sb("brow", [1, 6 * D])
            Rbrow = Res("brow")
            mrow = sb("mrow", [1, NB, 6 * D])
            Rmrow = Res("mrow")
            c0 = S_.chan("p0a")
            S_.dma("sp", c0, scb[:], cT[:, :, :], writes=[Rscb])
            S_.dma("sp", c0, brow[:], b_ada[:, :], writes=[Rbrow])
            op("act", lambda e: e.activation(out=scb[:], in_=scb[:], func=AF.Silu), reads=[Rscb], writes=[Rscb])
            li = 0
            for cg in range(6):
                for k in range(8):
                    j = li % 3
                    li += 1
                    S_.dma("sp", cstg[j], stg[j][:], w_ada[k * 128:(k + 1) * 128, cg * 1024:(cg + 1) * 1024],
                           writes=[Rstg[j]])
                    for b in range(NB):
                        for hf in range(2):
                            bk = b * 2 + hf
                            op("pe", lambda e, bk=bk, b=b, k=k, j=j, hf=hf: e.matmul(
                                B[bk][0:1, :], scb[:, b, k:k + 1], stg[j][:, hf * 512:(hf + 1) * 512],
                                start=(k == 0), stop=(k == 7), skip_group_check=True),
                               reads=[Rscb, Rstg[j]], writes=[RB[bk]])
                for b in range(NB):
                    for hf in range(2):
                        bk = b * 2 + hf
                        c0_ = cg * 1024 + hf * 512
                        op("dve", lambda e, bk=bk, b=b, c0_=c0_: e.tensor_tensor(
                            out=mrow[0:1, b, c0_:c0_ + 512], in0=B[bk][0:1, :], in1=brow[0:1, c0_:c0_ + 512],
                            op=ALU.add), reads=[RB[bk], Rbrow], writes=[Rmrow])
            S_.dma("sp", c0, modrow.rearrange("(o b) n -> o b n", o=1), mrow[:], reads=[Rmrow])
            NCV = 6
            cvin = [sb("cvin%d" % i, [128, 2 * D]) for i in range(NCV)]
            cvout = [sb("cvout%d" % i, [128, 2 * D], BF16) for i in range(NCV)]
            Rcvin = [Res("cvin%d" % i) for i in range(NCV)]
            Rcvout = [Res("cvout%d" % i) for i in range(NCV)]
            ccvu = [S_.chan("cvu%d" % i) for i in range(NCV)]
            ccvv = [S_.chan("cvv%d" % i) for i in range(NCV)]
            ccvs = [S_.chan("cvs%d" % i) for i in range(NCV)]
            def cv_load(r):
                k = r % NCV
                S_.dma("sp", ccvu[k], cvin[k][:, 0:D], pu[r * 128:(r + 1) * 128, :], writes=[Rcvin[k]])
                S_.dma("sp", ccvv[k], cvin[k][:, D:2 * D], pv[r * 128:(r + 1) * 128, :], writes=[Rcvin[k]])
            for r in range(NCV - 1):
                cv_load(r)
            for r in range(128):
                k = r % NCV
                if r + NCV - 1 < 128:
                    cv_load(r + NCV - 1)
                eng = ("dve", "act", "dve", "dve", "act", "pool")[r % 6]
                if eng == "act":
                    op("act", lambda e: e.activation(out=cvout[k][:], in_=cvin[k][:], func=AF.Copy),
                       reads=[Rcvin[k]], writes=[Rcvout[k]])
                else:
                    op(eng, lambda e: e.tensor_copy(out=cvout[k][:], in_=cvin[k][:]), reads=[Rcvin[k]],
                       writes=[Rcvout[k]])
                S_.dma("act", ccvs[k], uvd[r * 128:(r + 1) * 128, :], cvout[k][:], reads=[Rcvout[k]])
            S_.barrier(bres)
            S_.emit()

        with ExitStack() as pa:
            tot = [0]

            def sb(name, shape, dt=F32):
                n = int(np.prod(shape[1:])) * (4 if dt in (F32, U32, I32) else 2)
                tot[0] += n
                return pa.enter_context(nc.sbuf_tensor("a_" + name, list(shape), dt))
            NXB = 2
            win = sb("win", [128, 8, IN_COLS], BF16)
            wba = sb("wba", [128, 4, D], BF16)
            wbb = sb("wbb", [128, 4, D], BF16)
            wout = sb("wout", [128, 8, D], BF16)
            consts = sb("consts", [128, 6, 128])
            identb = sb("identb", [128, 128], BF16)
            EB = sb("EB", [128, 4, 8, 128], BF16)
            A1 = sb("A1", [128, D])
            sh1 = sb("sh1", [128, D])
            gt1 = sb("gt1", [128, D])
            gmn = sb("gmn", [128, 512])
            cw = sb("cw", [128, 8, 4])
            cb = sb("cb", [128, 8])
            bifb = sb("bifb", [128, 8])
            cst = sb("cst", [128, 4])
            nh4 = sb("nh4", [128, 4])
            xb = [sb("xb%d" % i, [128, D]) for i in range(NXB)]
            tmp = sb("tmp", [128, D])
            hT = sb("hT", [128, 8, 128], BF16)
            qTz = sb("qTz", [128, 4, 2, 128], BF16)
            kTh = sb("kTh", [128, 4, 5, 128], BF16)
            vh = sb("vh", [128, 5, 8, 65], BF16)
            ub = sb("ub", [128, 8, 131])
            cacc = [sb("cacc%d" % i, [128, 128]) for i in range(2)]
            qbT = sb("qbT", [128, 4, 128], BF16)
            kbT = sb("kbT", [128, 4, 128], BF16)
            zq = sb("zq", [128, 4, 2, 128], BF16)
            sga = sb("sga", [128, 8, 128], BF16)
            sgb = sb("sgb", [128, 8, 128], BF16)
            vba = sb("vba", [128, 4, 129], BF16)
            sob = sb("sob", [128, 512])
            gsb = sob
            gsm = sb("gsm", [128, 8])
            PTa = [sb("PTa%d" % i, [128, 5, 2, 128], BF16) for i in range(2)]
            ya = sb("ya", [128, 512], BF16)
            yaT = sb("yaT", [128, 4, 128], BF16)
            yb = sb("yb", [128, 512], BF16)
            ybT = sb("ybT", [128, 4, 128], BF16)
            Cst = sb("Cst", [128, 4, 129])
            Cbf = [sb("Cbf%d" % i, [128, 4, 129], BF16) for i in range(2)]
            kwz = sb("kwz", [128, 2, 4, 128], BF16)
            PTm = sb("PTm", [128, 4, 128], BF16)
            yT = sb("yT", [128, 8, 128], BF16)
            st = sb("st", [128, 64])
            print("phaseA sbuf bytes/partition:", tot[0])

            Rw = Res("weights")
            Rconst = Res("consts")
            Rmod = Res("mod")
            Rxb = [Res("xb%d" % i) for i in range(NXB)]
            cxb = [S_.chan("xb%d" % i) for i in range(NXB)]
            cxs = [S_.chan("xs%d" % i) for i in range(NXB)]
            cmisc = S_.chan("misc")
            cmisc2 = S_.chan("misc2")
            Rtmp = Res("tmp"); RhT = Res("hT"); RqT = Res("qT")
            RkTh = [Res("kTh%d" % i) for i in range(5)]
            Rvh = [Res("vh%d" % i) for i in range(5)]
            Rub = Res("ub"); Rcacc = [Res("cacc0"), Res("cacc1")]
            RqbT = Res("qbT"); RkbT = Res("kbT"); Rzq = Res("zq"); Rsga = Res("sga"); Rsgb = Res("sgb")
            Rvba = Res("vba"); Rsob = Res("sob"); Rgsb = Rsob; Rgsm = Res("gsm")
            RPTa = [Res("PTa0"), Res("PTa1")]
            Rya = Res("ya"); RyaT = Res("yaT"); Ryb = Res("yb"); RybT = Res("ybT")
            RC = Res("Cst"); RCbf = [Res("Cbf0"), Res("Cbf1")]
            Rkw = Res("kw"); RPTm = Res("PTm"); RyT = Res("yT")
            Rst = Res("st"); Rst2 = Res("st2"); Rst3 = Res("st3")
            hbf = yT[:].rearrange("p k t -> p (k t)")
            Rhbf = RyT
            hb = tmp[:, 512:1024].rearrange("p (h d) -> p h d", h=4)
            Rhb = Rtmp

            S_.dma("sp", cmisc, consts[:], consts_d[:, :, :], writes=[Rconst])
            S_.dma("sp", cmisc, cw[:], cwT[:, :, :], writes=[Rconst])
            S_.dma("sp", cmisc, cb[:], cbT[:, :], writes=[Rconst])
            S_.dma("sp", cmisc, bifb[:], bif[0:1, :].partition_broadcast(128), writes=[Rconst])
            S_.dma("sp", cmisc, gmn[:], gmn_d[0:1, :].partition_broadcast(128), writes=[Rconst])
            op("dve", lambda e: e.tensor_copy(out=identb[:], in_=consts[:, 0, :]), reads=[Rconst], writes=[Rconst])
            op("pool", lambda e: e.memset(cst[:, 0:1], EPS), writes=[Rconst])
            op("pool", lambda e: e.memset(nh4[:], -0.5), writes=[Rconst])
            op("pool", lambda e: e.memset(cst[:, 1:2], 1.0), writes=[Rconst])
            op("pool", lambda e: e.memset(cst[:, 2:3], float(np.log(128.0 ** -0.5))), writes=[Rconst])
            op("pool", lambda e: e.memset(vh[:], 1.0), writes=Rvh)
            op("pool", lambda e: e.memset(vba[:], 1.0), writes=[Rvba])
            op("pool", lambda e: e.memset(zq[:], 0.0), writes=[Rzq])
            op("pool", lambda e: e.memset(qTz[:], 0.0), writes=[RqT])
            op("pool", lambda e: e.memset(kwz[:], 0.0), writes=[Rkw])
            cvt_i = [0]

            def load_cast(dst, src, ncols):
                j = cvt_i[0] % NXB
                engs = ("dve", "act", "pool")
                eng = engs[cvt_i[0] % 3]
                cvt_i[0] += 1
                S_.dma("sp", cxb[j], xb[j][:, 0:ncols], src, writes=[Rxb[j]])
                if eng == "act":
                    op("act", lambda e: e.activation(out=dst, in_=xb[j][:, 0:ncols], func=AF.Copy),
                       reads=[Rxb[j]], writes=[Rw])
                else:
                    op(eng, lambda e: e.tensor_copy(out=dst, in_=xb[j][:, 0:ncols]), reads=[Rxb[j]], writes=[Rw])
            for k in range(8):
                for c0_ in range(0, IN_COLS, 1024):
                    n = min(1024, IN_COLS - c0_)
                    load_cast(win[:, k, c0_:c0_ + n], w_in[k * 128:(k + 1) * 128, c0_:c0_ + n], n)
            for k in range(4):
                load_cast(wba[:, k, :], wba_d[k * 128:(k + 1) * 128, :], 1024)
                load_cast(wbb[:, k, :], wbb_d[k * 128:(k + 1) * 128, :], 1024)
            for k in range(8):
                load_cast(wout[:, k, :], wout_d[k * 128:(k + 1) * 128, :], 1024)
            for rc in range(4):
                j = cvt_i[0] % NXB
                cvt_i[0] += 1
                S_.dma("sp", cxb[j], xb[j][:], ebsrc[:, rc, :, :].rearrange("p h l -> p (h l)"), writes=[Rxb[j]])
                op("act", lambda e, j=j, rc=rc: e.activation(
                    out=EB[:, rc, :, :].rearrange("p h l -> p (h l)"), in_=xb[j][:], func=AF.Exp),
                   reads=[Rxb[j]], writes=[Rconst])
            op("pool", lambda e: e.memset(EB[64:128, 0, :, 0:64], 0.0), writes=[Rconst])
            op("pool", lambda e: e.memset(EB[0:64, 3, :, 64:128], 0.0), writes=[Rconst])

            def slot(i):
                return i % 5

            cpar_box = [0]
            RmodG = Res('modG')

            def sa1(b, i):
                g = b * NT + i
                j = g % NXB
                row0 = g * 128
                sl = slot(i)
                if i == 0:
                    S_.dma('sp', cmisc, A1[:], g1[0:1, :].partition_broadcast(128), writes=[Rmod])
                    S_.dma('sp', cmisc, tmp[:], modrow[b:b + 1, D:2 * D].partition_broadcast(128), writes=[Rtmp])
                    S_.dma('sp', cmisc, sh1[:], modrow[b:b + 1, 0:D].partition_broadcast(128), writes=[Rmod])
                    op('dve', lambda e: e.scalar_tensor_tensor(out=A1[:], in0=tmp[:], scalar=1.0, in1=A1[:],
                                                               op0=ALU.add, op1=ALU.mult),
                       reads=[Rtmp, Rmod], writes=[Rmod])
                    op('pool', lambda e: e.memset(ub[:, :, 0:3], 0.0), writes=[Rub])
                if g == 0:
                    S_.dma("sp", cxb[j], xb[j][:], x[row0:row0 + 128, :], writes=[Rxb[j]])
                op("act", lambda e, j=j: e.activation(out=tmp[:], in_=xb[j][:], func=AF.Square, scale=1.0 / 32,
                                                      accum_out=st[:, 0:1]), reads=[Rxb[j]], writes=[Rtmp, Rst])
                op("pool", lambda e: e.tensor_scalar(out=st[:, 1:2], in0=st[:, 0:1], scalar1=EPS, scalar2=None,
                                                     op0=ALU.add), reads=[Rst], writes=[Rst])
                op("pool", lambda e: e.tensor_tensor(out=st[:, 2:3], in0=st[:, 1:2], in1=nh4[:, 0:1], op=ALU.pow),
                   reads=[Rst, Rconst], writes=[Rst])
                op("dve", lambda e, j=j: e.scalar_tensor_tensor(out=tmp[:], in0=xb[j][:], scalar=st[:, 2:3],
                                                                in1=A1[:], op0=ALU.mult, op1=ALU.mult),
                   reads=[Rxb[j], Rst, Rmod], writes=[Rtmp])
                op("pool", lambda e: e.tensor_tensor(out=hbf, in0=tmp[:], in1=sh1[:], op=ALU.add),
                   reads=[Rtmp, Rmod], writes=[Rhbf])
                for k in range(8):
                    op("pe", lambda e, k=k: e.transpose(out=Tb[0][:, k * 128:(k + 1) * 128],
                                                        in_=hbf[:, k * 128:(k + 1) * 128], identity=identb[:]),
                       reads=[Rhbf, Rconst], writes=[RT[0]])
                op("act", lambda e: e.activation(out=hT[:].rearrange("p k t -> p (k t)"), in_=Tb[0][:],
                                                 func=AF.Copy), reads=[RT[0]], writes=[RhT])

                yield 'p1'
                pbank = [0]

                def fm_group(col0, evac):
                    bk = pbank[0] % 2
                    pbank[0] += 1
                    for m in range(4):
                        for k in range(8):
                            op("pe", lambda e, bk=bk, m=m, k=k: e.matmul(
                                B[bk][:, m * 128:(m + 1) * 128],
                                win[:, k, col0 + m * 128:col0 + (m + 1) * 128], hT[:, k, :],
                                start=(k == 0), stop=(k == 7), skip_group_check=True),
                               reads=[Rw, RhT], writes=[RB[bk]])
                    evac(bk)

                def tm_group(col0, ncol, evac):
                    bk = pbank[0] % 2
                    pbank[0] += 1
                    for k in range(8):
                        op("pe", lambda e, bk=bk, k=k: e.matmul(
                            B[bk][:, 0:ncol], hT[:, k, :], win[:, k, col0:col0 + ncol],
                            start=(k == 0), stop=(k == 7), skip_group_check=True),
                           reads=[Rw, RhT], writes=[RB[bk]])
                    evac(bk)

                def evac_q(bk):
                    for e_ in range(2):
                        op("act", lambda e: e.activation(
                            out=qTz[64 * e_:64 * e_ + 64, :, e_, :],
                            in_=B[bk][64 * e_:64 * e_ + 64, :].rearrange("p (m t) -> p m t", m=4),
                            func=AF.Copy, scale=0.125), reads=[RB[bk]], writes=[RqT])
                fm_group(0, evac_q)
                yield 'p1'
                fm_group(512, lambda bk: op("dve", lambda e: e.tensor_copy(
                    out=kTh[:, :, sl, :], in_=B[bk][:].rearrange("p (m t) -> p m t", m=4)),
                    reads=[RB[bk]], writes=[RkTh[sl]]))
                yield 'p1'
                tm_group(1024, 512, lambda bk: op("dve", lambda e: e.tensor_copy(
                    out=vh[:, sl, :, 0:64], in_=B[bk][:].rearrange("p (h d) -> p h d", h=8)),
                    reads=[RB[bk]], writes=[Rvh[sl]]))
                yield 'P1END'
                fm_group(1536, lambda bk: op("act", lambda e: e.activation(
                    out=ub[:, 0:4, 3:131], in_=B[bk][:].rearrange("p (m t) -> p m t", m=4), func=AF.Copy),
                    reads=[RB[bk]], writes=[Rub]))
                yield 'p2'
                fm_group(2048, lambda bk: op("dve", lambda e: e.tensor_copy(
                    out=ub[:, 4:8, 3:131], in_=B[bk][:].rearrange("p (m t) -> p m t", m=4)),
                    reads=[RB[bk]], writes=[Rub]))
                yield 'p2'
                tm_group(2560, 512, lambda bk: op("dve", lambda e: e.tensor_copy(
                    out=vba[:, :, 0:128], in_=B[bk][:].rearrange("p (h d) -> p h d", h=4)),
                    reads=[RB[bk]], writes=[Rvba]))
                yield 'p2'
                tm_group(3072, 512, lambda bk: op("act", lambda e: e.activation(
                    out=sob[:], in_=B[bk][:], func=AF.Sigmoid), reads=[RB[bk]], writes=[Rsob]))
                op("pool", lambda e: e.tensor_tensor(out=gsb[:], in0=sob[:], in1=gmn[:], op=ALU.mult),
                   reads=[Rsob, Rconst], writes=[Rsob])
                yield 'p2'
                tm_group(3584, 8, lambda bk: op("dve", lambda e: e.tensor_tensor(
                    out=gsm[:], in0=B[bk][:, 0:8], in1=bifb[:], op=ALU.add),
                    reads=[RB[bk], Rconst], writes=[Rgsm]))
                yield 'p2'
                yield 'P2END'
                for q in range(2):
                    fm_group(3592 + q * 512, lambda bk, q=q: op("act", lambda e: e.activation(
                        out=sga[:, 4 * q:4 * q + 4, :].rearrange("p m t -> p (m t)"), in_=B[bk][:],
                        func=AF.Sigmoid), reads=[RB[bk]], writes=[Rsga]))
                    yield 'p3'
                for q in range(2):
                    fm_group(4616 + q * 512, lambda bk, q=q: op("act", lambda e: e.activation(
                        out=sgb[:, 4 * q:4 * q + 4, :].rearrange("p m t -> p (m t)"), in_=B[bk][:],
                        func=AF.Sigmoid), reads=[RB[bk]], writes=[Rsgb]))
                    yield 'p3'
                for m in range(8):
                    ca = cacc[m % 2]
                    Rca = Rcacc[m % 2]
                    op("dve", lambda e, m=m, ca=ca: e.tensor_scalar(
                        out=ca[:], in0=ub[:, m, 3:131], scalar1=cw[:, m, 3:4], scalar2=cb[:, m:m + 1],
                        op0=ALU.mult, op1=ALU.add), reads=[Rub, Rconst], writes=[Rca])
                    for tp in range(3):
                        op("dve", lambda e, m=m, ca=ca, tp=tp: e.scalar_tensor_tensor(
                            out=ca[:], in0=ub[:, m, tp:tp + 128], scalar=cw[:, m, tp:tp + 1], in1=ca[:],
                            op0=ALU.mult, op1=ALU.add), reads=[Rub, Rconst, Rca], writes=[Rca])
                    if m < 4:
                        op("act", lambda e, m=m, ca=ca: e.activation(out=qbT[:, m, :], in_=ca[:], func=AF.Silu),
                           reads=[Rca], writes=[RqbT])
                    else:
                        op("act", lambda e, m=m, ca=ca: e.activation(out=kbT[:, m - 4, :], in_=ca[:],
                                                                     func=AF.Silu),
                           reads=[Rca], writes=[RkbT])
                    yield 'p3'
                op("pool", lambda e: e.tensor_copy(out=ub[:, :, 0:3], in_=ub[:, :, 128:131]),
                   reads=[Rub], writes=[Rub])
                op("pool", lambda e: e.tensor_copy(out=zq[:, :, 0, 0:64], in_=qbT[:, :, 0:64]),
                   reads=[RqbT], writes=[Rzq])
                op("pool", lambda e: e.tensor_copy(out=zq[:, :, 1, 64:128], in_=qbT[:, :, 64:128]),
                   reads=[RqbT], writes=[Rzq])
                yield 'p3'

            def sa2(b, i):
                g = b * NT + i
                j = g % NXB
                row0 = g * 128
                if i == 0:
                    S_.dma('sp', cmisc2, gt1[:], modrow[b:b + 1, 2 * D:3 * D].partition_broadcast(128), writes=[RmodG])
                    op('pool', lambda e: e.memset(Cst[:], 0.0), writes=[RC])
                    op('pool', lambda e: e.memset(Cbf[0][:], 0.0), writes=[RCbf[0]])
                    cpar_box[0] = 0
                cpar = cpar_box[0]
                if g + 1 < NB * NT:
                    jn = (g + 1) % NXB
                    S_.dma("sp", cxb[jn], xb[jn][:], x[row0 + 128:row0 + 256, :], writes=[Rxb[jn]])
                nr = min(i, 4) + 1
                rcs = (0, 1, 2, 2, 3)
                def st_part(hp):
                    pt = PTa[hp % 2]
                    Rpt = RPTa[hp % 2]
                    bk0 = 2 if hp % 2 == 0 else 0
                    for r0 in range(0, nr, 2):
                        bk = bk0 + (r0 // 2) % 2
                        rr = [r for r in (r0, r0 + 1) if r < nr]
                        for r in rr:
                            ks = slot(i - r)
                            for e_ in range(2):
                                op("pe", lambda e, bk=bk, r=r, r0=r0, ks=ks, e_=e_, hp=hp: e.matmul(
                                    B[bk][:, (r - r0) * 256 + e_ * 128:(r - r0) * 256 + (e_ + 1) * 128],
                                    kTh[:, hp, ks, :], qTz[:, hp, e_, :],
                                    start=True, stop=True, skip_group_check=True),
                                   reads=[RkTh[ks], RqT], writes=[RB[bk]])
                        nn = len(rr)
                        op("act", lambda e, bk=bk, r0=r0, nn=nn, pt=pt: e.activation(
                            out=pt[:, r0:r0 + nn, :, :].rearrange("p r e t -> p (r e t)"),
                            in_=B[bk][:, 0:nn * 256], func=AF.Exp), reads=[RB[bk]], writes=[Rpt])
                    for r in range(nr):
                        op("dve", lambda e, r=r, pt=pt, hp=hp: e.tensor_tensor(
                            out=pt[:, r, :, :], in0=pt[:, r, :, :], in1=EB[:, rcs[r], 2 * hp:2 * hp + 2, :],
                            op=ALU.mult), reads=[Rpt, Rconst], writes=[Rpt])

                def pv_part(hp):
                    pt = PTa[hp % 2]
                    Rpt = RPTa[hp % 2]
                    for e_ in range(2):
                        h = 2 * hp + e_
                        bk = 4 + h // 4
                        for r in range(nr):
                            ks = slot(i - r)
                            op("pe", lambda e, bk=bk, h=h, r=r, ks=ks, e_=e_, pt=pt: e.matmul(
                                B[bk][:, (h % 4) * 65:(h % 4) * 65 + 65], pt[:, r, e_, :], vh[:, ks, h, :],
                                start=(r == 0), stop=(r == nr - 1), skip_group_check=True),
                               reads=[Rpt, Rvh[ks]], writes=[RB[bk]])

                st_part(0)
                for hp in range(4):
                    if hp + 1 < 4:
                        st_part(hp + 1)
                    pv_part(hp)
                for q in range(2):
                    bk = 4 + q
                    pv_ = B[bk][:, 0:260].rearrange("p (h d) -> p h d", h=4)
                    op("dve", lambda e, q=q, pv_=pv_: e.reciprocal(out=st[:, 8 + 4 * q:12 + 4 * q],
                                                                  in_=pv_[:, :, 64]),
                       reads=[RB[bk]], writes=[Rst2])
                    op("dve", lambda e, q=q, pv_=pv_: e.tensor_tensor(
                        out=ya[:, 256 * q:256 * q + 256].rearrange("p (h d) -> p h d", h=4),
                        in0=pv_[:, :, 0:64],
                        in1=st[:, 8 + 4 * q:12 + 4 * q].unsqueeze(2).to_broadcast([128, 4, 64]),
                        op=ALU.mult), reads=[RB[bk], Rst2], writes=[Rya])
                for c_ in range(4):
                    op("pe", lambda e, c_=c_: e.transpose(out=Tb[1][:, 512 + c_ * 128:512 + (c_ + 1) * 128],
                                                          in_=ya[:, c_ * 128:(c_ + 1) * 128], identity=identb[:]),
                       reads=[Rya, Rconst], writes=[RT[1]])
                op("act", lambda e: e.activation(out=yaT[:].rearrange("p c t -> p (c t)"), in_=Tb[1][:, 512:1024],
                                                 func=AF.Copy), reads=[RT[1]], writes=[RyaT])

                yield 'Q1END'
                for h in range(4):
                    op("pe", lambda e, h=h: e.transpose(out=Tb[1][:, h * 128:(h + 1) * 128], in_=kbT[:, h, :],
                                                        identity=identb[:]),
                       reads=[RkbT, Rconst], writes=[RT[1]])
                for h in range(4):
                    op("pe", lambda e, h=h: e.matmul(B[1][:, h * 128:(h + 1) * 128], kbT[:, h, :], qbT[:, h, :],
                                                     start=True, stop=True, skip_group_check=True),
                       reads=[RkbT, RqbT], writes=[RB[1]])
                op("act", lambda e: e.activation(out=st[:, 16:20], in_=gsm[:, 4:8], func=AF.Exp, scale=-1.0),
                   reads=[Rgsm], writes=[Rst3])
                op("act", lambda e: e.activation(out=st[:, 20:24], in_=st[:, 16:20], func=AF.Ln, bias=cst[:, 1:2]),
                   reads=[Rst3, Rconst], writes=[Rst3])
                for q in range(4):
                    op("pe", lambda e, q=q: e.matmul(B[0][:, 4 * q:4 * q + 4], consts[:, 1 + q, :], st[:, 20:24],
                                                     start=(q == 0), stop=(q == 3), skip_group_check=True),
                       reads=[Rconst, Rst3], writes=[RB[0]])
                op("dve", lambda e: e.tensor_tensor(out=st[:, 24:28], in0=gsm[:, 0:4], in1=B[0][:, 0:4], op=ALU.add),
                   reads=[Rgsm, RB[0]], writes=[Rst3])
                op("dve", lambda e: e.tensor_tensor(out=st[:, 28:32], in0=gsm[:, 0:4], in1=B[0][:, 4:8],
                                                    op=ALU.subtract), reads=[Rgsm, RB[0]], writes=[Rst3])
                op("act", lambda e: e.activation(out=st[:, 32:36], in_=B[0][:, 0:4], func=AF.Exp, scale=-1.0),
                   reads=[RB[0]], writes=[Rst3])
                op("act", lambda e: e.activation(out=st[:, 36:44], in_=st[:, 24:32], func=AF.Exp, bias=cst[:, 2:3]),
                   reads=[Rst3, Rconst], writes=[Rst3])
                op("act", lambda e: e.activation(out=st[:, 44:52], in_=B[0][:, 8:16], func=AF.Exp, scale=-1.0),
                   reads=[RB[0]], writes=[Rst3])
                yield 'q2'
                for h in range(4):
                    for c_ in range(2):
                        ps_ = slice(64 * c_, 64 * c_ + 64)
                        op("dve", lambda e: e.tensor_scalar(out=kwz[ps_, c_, h, :],
                                                            in0=Tb[1][ps_, h * 128:(h + 1) * 128],
                                                            scalar1=st[ps_, 40 + h:41 + h], scalar2=None,
                                                            op0=ALU.mult),
                           reads=[RT[1], Rst3], writes=[Rkw])
                yield 'q2'
                for h in range(4):
                    op("dve", lambda e, h=h: e.scalar_tensor_tensor(
                        out=PTm[:, h, :], in0=B[1][:, h * 128:(h + 1) * 128], scalar=st[:, 36 + h:37 + h],
                        in1=consts[:, 1, :], op0=ALU.mult, op1=ALU.mult),
                       reads=[RB[1], Rst3, Rconst], writes=[RPTm])
                yield 'q2'
                for h in range(4):
                    bk = 2 + h // 2
                    reg = B[bk][:, (h % 2) * 129:(h % 2) * 129 + 129]
                    op("pe", lambda e, reg=reg, h=h: e.matmul(reg, PTm[:, h, :], vba[:, h, :],
                                                              start=(h % 2 == 0), stop=False,
                                                              skip_group_check=True),
                       reads=[RPTm, Rvba], writes=[RB[bk]])
                for h in range(4):
                    bk = 2 + h // 2
                    reg = B[bk][:, (h % 2) * 129:(h % 2) * 129 + 129]
                    op("pe", lambda e, reg=reg, h=h, cp=cpar: e.matmul(reg, zq[:, h, 0, :], Cbf[cp][:, h, :],
                                                                       start=False, stop=False,
                                                                       skip_group_check=True),
                       reads=[Rzq, RCbf[cpar]], writes=[RB[bk]])
                yield 'q2'
                for c_ in range(2):
                    for h in range(4):
                        bk = 4 + h // 2
                        reg = B[bk][:, (h % 2) * 129:(h % 2) * 129 + 129]
                        op("pe", lambda e, reg=reg, h=h, c_=c_: e.matmul(
                            reg, kwz[:, c_, h, :], vba[:, h, :],
                            start=(h % 2 == 0), stop=True, skip_group_check=True),
                           reads=[Rkw, Rvba], writes=[RB[bk]])
                    for h in range(4):
                        bk = 4 + h // 2
                        reg = B[bk][:, (h % 2) * 129:(h % 2) * 129 + 129]
                        op("dve", lambda e, reg=reg, h=h, c_=c_: e.scalar_tensor_tensor(
                            out=Cst[:, h, :], in0=Cst[:, h, :], scalar=st[:, 44 + 4 * c_ + h:45 + 4 * c_ + h],
                            in1=reg, op0=ALU.mult, op1=ALU.add),
                           reads=[RC, Rst3, RB[bk]], writes=[RC])
                    npar = 1 - cpar
                    op("pool", lambda e, npar=npar: e.tensor_copy(out=Cbf[npar][:], in_=Cst[:]),
                       reads=[RC], writes=[RCbf[npar]])
                    cpar = npar
                    if c_ == 0:
                        for h in range(4):
                            bk = 2 + h // 2
                            reg = B[bk][:, (h % 2) * 129:(h % 2) * 129 + 129]
                            op("pe", lambda e, reg=reg, h=h, cp=cpar: e.matmul(
                                reg, zq[:, h, 1, :], Cbf[cp][:, h, :], start=False, stop=True,
                                skip_group_check=True),
                               reads=[Rzq, RCbf[cpar]], writes=[RB[bk]])
                    yield 'q2'
                for q in range(2):
                    bk = 2 + q
                    nv = B[bk][:, 0:258].rearrange("p (h d) -> p h d", h=2)
                    op("dve", lambda e, q=q, nv=nv: e.tensor_tensor(
                        out=st[:, 52 + 2 * q:54 + 2 * q], in0=nv[:, :, 128], in1=st[:, 32 + 2 * q:34 + 2 * q],
                        op=ALU.mult), reads=[RB[bk], Rst3], writes=[Rst3])
                op("dve", lambda e: e.tensor_scalar(out=st[:, 4:8], in0=st[:, 52:56], scalar1=-1.0, scalar2=None,
                                                    op0=ALU.mult), reads=[Rst3], writes=[Rst3])
                op("dve", lambda e: e.scalar_tensor_tensor(out=st[:, 52:56], in0=st[:, 52:56], scalar=1.0,
                                                           in1=st[:, 4:8], op0=ALU.max, op1=ALU.max),
                   reads=[Rst3], writes=[Rst3])
                op("dve", lambda e: e.reciprocal(out=st[:, 52:56], in_=st[:, 52:56]), reads=[Rst3], writes=[Rst3])
                op("dve", lambda e: e.tensor_tensor(out=st[:, 56:60], in0=st[:, 32:36], in1=st[:, 52:56],
                                                    op=ALU.mult), reads=[Rst3], writes=[Rst3])
                for q in range(2):
                    bk = 2 + q
                    nv = B[bk][:, 0:258].rearrange("p (h d) -> p h d", h=2)
                    op("dve", lambda e, q=q, nv=nv: e.tensor_tensor(
                        out=hb[:, 2 * q:2 * q + 2, :], in0=nv[:, :, 0:128],
                        in1=st[:, 56 + 2 * q:58 + 2 * q].unsqueeze(2).to_broadcast([128, 2, 128]),
                        op=ALU.mult), reads=[RB[bk], Rst3], writes=[Rhb])
                tmpv = tmp[:, 0:512].rearrange("p (h d) -> p h d", h=4)
                op("pool", lambda e: e.tensor_tensor(out=tmpv, in0=hb, in1=hb, op=ALU.mult),
                   reads=[Rhb], writes=[Rtmp])
                op("dve", lambda e: e.tensor_reduce(out=st[:, 60:64], in_=tmpv, axis=AX.X, op=ALU.add),
                   reads=[Rtmp], writes=[Rst3])
                op("pool", lambda e: e.tensor_scalar(out=st[:, 4:8], in0=st[:, 60:64], scalar1=1.0 / 128, scalar2=EPS,
                                                     op0=ALU.mult, op1=ALU.add), reads=[Rst3], writes=[Rst3])
                op("pool", lambda e: e.tensor_tensor(out=st[:, 60:64], in0=st[:, 4:8], in1=nh4[:], op=ALU.pow),
                   reads=[Rst3, Rconst], writes=[Rst3])
                op("dve", lambda e: e.tensor_tensor(
                    out=tmpv, in0=hb, in1=st[:, 60:64].unsqueeze(2).to_broadcast([128, 4, 128]), op=ALU.mult),
                   reads=[Rhb, Rst3], writes=[Rtmp])
                op("pool", lambda e: e.tensor_tensor(out=yb[:], in0=tmp[:, 0:512], in1=gsb[:], op=ALU.mult),
                   reads=[Rtmp, Rgsb], writes=[Ryb])
                for c_ in range(4):
                    op("pe", lambda e, c_=c_: e.transpose(out=Tb[1][:, 512 + c_ * 128:512 + (c_ + 1) * 128],
                                                          in_=yb[:, c_ * 128:(c_ + 1) * 128], identity=identb[:]),
                       reads=[Ryb, Rconst], writes=[RT[1]])
                op("act", lambda e: e.activation(out=ybT[:].rearrange("p c t -> p (c t)"), in_=Tb[1][:, 512:1024],
                                                 func=AF.Copy), reads=[RT[1]], writes=[RybT])

                cpar_box[0] = cpar
                yield 'Q2END'
                for q in range(2):
                    for n in range(4):
                        cn = (4 * q + n) * 128
                        for kc in range(4):
                            op("pe", lambda e, q=q, n=n, kc=kc, cn=cn: e.matmul(
                                B[q][:, n * 128:(n + 1) * 128], wba[:, kc, cn:cn + 128], yaT[:, kc, :],
                                start=(kc == 0), stop=(kc == 3), skip_group_check=True),
                               reads=[Rw, RyaT], writes=[RB[q]])
                    for n in range(4):
                        cn = (4 * q + n) * 128
                        for kc in range(4):
                            op("pe", lambda e, q=q, n=n, kc=kc, cn=cn: e.matmul(
                                B[2 + q][:, n * 128:(n + 1) * 128], wbb[:, kc, cn:cn + 128], ybT[:, kc, :],
                                start=(kc == 0), stop=(kc == 3), skip_group_check=True),
                               reads=[Rw, RybT], writes=[RB[2 + q]])
                    sgav = sga[:, 4 * q:4 * q + 4, :].rearrange("p m t -> p (m t)")
                    sgbv = sgb[:, 4 * q:4 * q + 4, :].rearrange("p m t -> p (m t)")
                    op("dve", lambda e, q=q, sgav=sgav: e.tensor_tensor(out=tmp[:, 0:512], in0=B[q][:], in1=sgav,
                                                                        op=ALU.mult),
                       reads=[RB[q], Rsga], writes=[Rtmp])
                    op("dve", lambda e, q=q, sgbv=sgbv: e.tensor_tensor(out=tmp[:, 512:1024], in0=B[2 + q][:],
                                                                        in1=sgbv, op=ALU.mult),
                       reads=[RB[2 + q], Rsgb], writes=[Rtmp])
                    op("pool", lambda e, q=q: e.tensor_tensor(
                        out=yT[:, 4 * q:4 * q + 4, :].rearrange("p m t -> p (m t)"), in0=tmp[:, 0:512],
                        in1=tmp[:, 512:1024], op=ALU.add), reads=[Rtmp], writes=[RyT])
                    yield 'q3'
                yield 'Q3END'
                for hf in range(2):
                    bk = 4 + hf
                    for k in range(8):
                        op("pe", lambda e, bk=bk, k=k, hf=hf: e.matmul(
                            B[bk][:], yT[:, k, :], wout[:, k, hf * 512:(hf + 1) * 512],
                            start=(k == 0), stop=(k == 7), skip_group_check=True),
                           reads=[RyT, Rw], writes=[RB[bk]])
                    op("dve", lambda e, bk=bk, hf=hf: e.tensor_tensor(
                        out=tmp[:, hf * 512:(hf + 1) * 512], in0=B[bk][:], in1=gt1[:, hf * 512:(hf + 1) * 512],
                        op=ALU.mult), reads=[RB[bk], RmodG], writes=[Rtmp])
                    yield 'q4'
                op("pool", lambda e, j=j: e.tensor_tensor(out=xb[j][:], in0=xb[j][:], in1=tmp[:], op=ALU.add),
                   reads=[Rtmp, Rxb[j]], writes=[Rxb[j]])
                dst = x1d if do_b else out
                S_.dma("sp", cxs[j], dst[row0:row0 + 128, :], xb[j][:], reads=[Rxb[j]])
                yield 'q4'

            tilesA = [(b, i) for b in range(NB) for i in range(NT)]
            for _ in sa1(*tilesA[0]):
                pass
            for n_, (b, i) in enumerate(tilesA):
                g2_ = sa2(b, i)
                g1_ = sa1(*tilesA[n_ + 1]) if n_ + 1 < len(tilesA) else iter(())
                while next(g2_) != 'Q1END':
                    pass
                for end2, end1 in (('Q2END', 'P1END'), ('Q3END', 'P2END')):
                    d2 = d1 = False
                    while not (d2 and d1):
                        if not d2:
                            d2 = (next(g2_) == end2)
                        if not d1:
                            d1 = (next(g1_, end1) == end1)
                d2 = d1 = False
                while not (d2 and d1):
                    if not d2:
                        d2 = (next(g2_, None) is None)
                    if not d1:
                        d1 = (next(g1_, None) is None)
            S_.barrier(bres)
            S_.emit()
        if do_b:
          with ExitStack() as pb:
            totb = [0]

            def sb(name, shape, dt=F32):
                n = int(np.prod(shape[1:])) * (4 if dt in (F32, U32, I32) else 2)
                totb[0] += n
                return pb.enter_context(nc.sbuf_tensor("b_" + name, list(shape), dt))
            NUV = 18
            ND = 4
            wpq = sb("wpq", [128, 8, 2048], BF16)
            keys = sb("keys", [128, 16, 128], BF16)
            identf = sb("identf", [128, 128])
            identb = sb("identb", [128, 128], BF16)
            iota = sb("iota", [128, 8, 16, 16])
            A2 = sb("A2", [128, D]); sh2 = sb("sh2", [128, D]); gt2 = sb("gt2", [128, D]); fgb = sb("fgb", [128, D])
            cst = sb("cst", [128, 4])
            xb = [sb("xb%d" % i, [128, D]) for i in range(3)]
            tmp = sb("tmp", [128, D])
            h2bf = [sb("h2bf%d" % i, [128, D], BF16) for i in range(2)]
            tmp2 = sb("tmp2", [128, D])
            prod = [sb("prod%d" % i, [128, D], BF16) for i in range(3)]
            st2 = sb("st2", [128, 16])
            h2T = sb("h2T", [128, 8, 128], BF16)
            qTp = sb("qTp", [128, 16, 128], BF16)
            sc = sb("sc", [128, 16, 128])
            scr = sb("scr", [128, 256])
            stop = sb("stop", [128, 16, 16])
            sidx = sb("sidx", [128, 16, 16], U32)
            sidxf = sb("sidxf", [128, 16, 16])
            cand = sb("cand", [128, 8, 16, 16])
            best = sb("best", [128, 8, 16])
            ci = sb("ci", [128, 8, 16], U32)
            hi = sb("hi", [128, 8, 16], U32)
            lo = sb("lo", [128, 8, 16], U32)
            hif = sb("hif", [128, 8, 16])
            lof = sb("lof", [128, 8, 16])
            oh = cand
            i1 = sb("i1", [128, 8, 16])
            i2 = sb("i2", [128, 8, 16])
            ef = sb("ef", [128, 8, 16])
            eidx = [sb("eidx%d" % i, [128, 128], U32) for i in range(2)]
            ge = sb("ge", [128, 8, 16])
            gate = [sb("gate%d" % i, [128, 8, 16]) for i in range(2)]
            gs = sb("gs", [128, 16])
            actv = sb("actv", [128, 128])
            gl = sb("gl", [128, 128])
            wv = sb("wv", [128, 128])
            UV = [sb("UV%d" % i, [128, 2 * D], BF16) for i in range(NUV)]
            dg = [sb("dg%d" % i, [128, 128], BF16) for i in range(ND)]
            st = sb("st", [128, 16])
            print("phaseB sbuf bytes/partition:", totb[0])
            Rw = Res("b_w"); Rconst = Res("b_const"); Rmod1 = Res("b_mod1"); Rmod2 = Res("b_mod2")
            Rxb = [Res("b_xb0"), Res("b_xb1"), Res("b_xb2")]
            cxb = [S_.chan("b_xb0"), S_.chan("b_xb1"), S_.chan("b_xb2")]
            cxs = [S_.chan("b_xs0"), S_.chan("b_xs1"), S_.chan("b_xs2")]
            cm = S_.chan("b_misc")
            cm2 = S_.chan("b_misc2")
            Rtmp = Res("b_tmp"); Rtmp2 = Res("b_tmp2"); Rst2 = Res("b_st2"); Rprod = [Res("prod0"), Res("prod1"), Res("prod2")]; Rh2bf = [Res("h2bf0"), Res("h2bf1")]; Rh2T = Res("h2T"); RqTp = Res("qTp")
            Rsc = Res("sc"); Rscr2 = [Res("scr0"), Res("scr1")]; Rstop = [Res("stop%d" % i) for i in range(16)]; Rsidx = [Res("sidx%d" % i) for i in range(16)]; Rsidxf = Res("sidxf")
            Rcand = Res("cand"); Rbest = [Res("best%d" % i) for i in range(8)]; Rci = [Res("ci%d" % i) for i in range(8)]; Rhl = Res("hl"); Roh = Rcand
            Ri12 = Res("i12"); Reidx = [Res("eidx0"), Res("eidx1")]; Rge = Res("ge"); Rgate = [Res("gate0"), Res("gate1")]; Rgs = Res("gs")
            Ractv = [Res("actv%d" % i) for i in range(128)]; Rgl = [Res("gl%d" % i) for i in range(4)]; Rwv = [Res("wv%d" % i) for i in range(4)]
            RUV = [Res("UV%d" % i) for i in range(NUV)]
            cUV = [S_.chan("UV%d" % i) for i in range(NUV)]
            cUVs = [S_.chan("UVs%d" % i) for i in range(NUV)]
            Ruvd = [Res("uvd%d" % i) for i in range(NUV)]
            Rdg = [Res("dg%d" % i) for i in range(ND)]
            Rst = Res("b_st")

            S_.dma("sp", cm, identf[:], consts_d[:, 0, :], writes=[Rconst])
            S_.dma("sp", cm, iota[:].rearrange("p a b c -> p (a b c)"), iota_d[:, :], writes=[Rconst])
            S_.dma("sp", cm, fgb[:], fg[0:1, :].partition_broadcast(128), writes=[Rconst])
            op("dve", lambda e: e.tensor_copy(out=identb[:], in_=identf[:]), reads=[Rconst], writes=[Rconst])
            op("pool", lambda e: e.memset(cst[:, 0:1], EPS), writes=[Rconst])
            ci_ = [0]

            def load_cast_b(dst, src, ncols):
                jx = ci_[0] % 3
                eng = ("dve", "act", "pool")[ci_[0] % 3]
                ci_[0] += 1
                S_.dma("sp", cxb[jx], xb[jx][:, 0:ncols], src, writes=[Rxb[jx]])
                if eng == "act":
                    op("act", lambda e: e.activation(out=dst, in_=xb[jx][:, 0:ncols], func=AF.Copy),
                       reads=[Rxb[jx]], writes=[Rw])
                else:
                    op(eng, lambda e: e.tensor_copy(out=dst, in_=xb[jx][:, 0:ncols]), reads=[Rxb[jx]], writes=[Rw])
            for k in range(8):
                for c0_ in range(0, 2048, 1024):
                    load_cast_b(wpq[:, k, c0_:c0_ + 1024], wpq_d[k * 128:(k + 1) * 128, c0_:c0_ + 1024], 1024)
            for q in range(2):
                load_cast_b(keys[:, 8 * q:8 * q + 8, :].rearrange("p g n -> p (g n)"),
                            keysT[:, 8 * q:8 * q + 8, :].rearrange("p g n -> p (g n)"), 1024)
            def stage1(b, i):
                g = b * NT + i
                j = g % 2
                jx = g % 3
                row0 = g * 128
                if i == 0:
                    S_.dma("sp", cm, A2[:], g2[0:1, :].partition_broadcast(128), writes=[Rmod1])
                    S_.dma("sp", cm, tmp[:], modrow[b:b + 1, 4 * D:5 * D].partition_broadcast(128), writes=[Rtmp])
                    S_.dma("sp", cm, sh2[:], modrow[b:b + 1, 3 * D:4 * D].partition_broadcast(128), writes=[Rmod1])
                    op("dve", lambda e: e.scalar_tensor_tensor(out=A2[:], in0=tmp[:], scalar=1.0, in1=A2[:],
                                                               op0=ALU.add, op1=ALU.mult),
                       reads=[Rtmp, Rmod1], writes=[Rmod1])
                S_.dma("sp", cxb[jx], xb[jx][:], x1d[row0:row0 + 128, :], writes=[Rxb[jx]])
                op("act", lambda e: e.activation(out=tmp[:], in_=xb[jx][:], func=AF.Square, scale=1.0 / 32,
                                                 accum_out=st[:, 0:1]), reads=[Rxb[jx]], writes=[Rtmp, Rst])
                op("act", lambda e: e.activation(out=st[:, 1:2], in_=st[:, 0:1], func=AF.Sqrt, bias=cst[:, 0:1]),
                   reads=[Rst, Rconst], writes=[Rst])
                op("dve", lambda e: e.reciprocal(out=st[:, 2:3], in_=st[:, 1:2]), reads=[Rst], writes=[Rst])
                op("dve", lambda e: e.scalar_tensor_tensor(out=tmp[:], in0=xb[jx][:], scalar=st[:, 2:3],
                                                           in1=A2[:], op0=ALU.mult, op1=ALU.mult),
                   reads=[Rxb[jx], Rst, Rmod1], writes=[Rtmp])
                op("dve", lambda e: e.tensor_tensor(out=h2bf[j][:], in0=tmp[:], in1=sh2[:], op=ALU.add),
                   reads=[Rtmp, Rmod1], writes=[Rh2bf[j]])
                for k in range(8):
                    op("pe", lambda e: e.transpose(out=Tb[0][:, k * 128:(k + 1) * 128],
                                                   in_=h2bf[j][:, k * 128:(k + 1) * 128], identity=identb[:]),
                       reads=[Rh2bf[j], Rconst], writes=[RT[0]])
                op("act", lambda e: e.activation(out=h2T[:].rearrange("p k t -> p (k t)"), in_=Tb[0][:],
                                                 func=AF.Copy), reads=[RT[0]], writes=[Rh2T])
                yield
                for q in range(4):
                    for m in range(4):
                        gq = 4 * q + m
                        for k in range(8):
                            op("pe", lambda e: e.matmul(B[q % 2][:, m * 128:(m + 1) * 128],
                                                        wpq[:, k, gq * 128:(gq + 1) * 128], h2T[:, k, :],
                                                        start=(k == 0), stop=(k == 7), skip_group_check=True),
                               reads=[Rw, Rh2T], writes=[RB[q % 2]])
                    op("act", lambda e: e.activation(out=qTp[:, 4 * q:4 * q + 4, :].rearrange("p g t -> p (g t)"),
                                                     in_=B[q % 2][:], func=AF.Copy), reads=[RB[q % 2]], writes=[RqTp])
                yield
                for q in range(4):
                    for m in range(4):
                        gq = 4 * q + m
                        op("pe", lambda e: e.matmul(B[q % 2][:, m * 128:(m + 1) * 128], qTp[:, gq, :], keys[:, gq, :],
                                                    start=True, stop=True, skip_group_check=True),
                           reads=[RqTp, Rw], writes=[RB[q % 2]])
                    op("act", lambda e: e.activation(out=sc[:, 4 * q:4 * q + 4, :].rearrange("p g n -> p (g n)"),
                                                     in_=B[q % 2][:], func=AF.Copy), reads=[RB[q % 2]], writes=[Rsc])
                yield
                for gq in range(16):
                    sq = scr[:, 128 * (gq % 2):128 * (gq % 2) + 128]
                    Rsq = Rscr2[gq % 2]
                    op("dve", lambda e: e.max(out=stop[:, gq, 0:8], in_=sc[:, gq, :]), reads=[Rsc], writes=[Rstop[gq]])
                    yield
                    op("dve", lambda e: e.max_index(out=sidx[:, gq, 0:8], in_max=stop[:, gq, 0:8],
                                                    in_values=sc[:, gq, :]), reads=[Rsc, Rstop[gq]], writes=[Rsidx[gq]])
                    yield
                    op("dve", lambda e: e.match_replace(out=sq, in_to_replace=stop[:, gq, 0:8],
                                                        in_values=sc[:, gq, :], imm_value=NEG),
                       reads=[Rsc, Rstop[gq]], writes=[Rsq])
                    yield
                    op("dve", lambda e: e.max(out=stop[:, gq, 8:16], in_=sq), reads=[Rsq],
                       writes=[Rstop[gq]])
                    yield
                    op("dve", lambda e: e.max_index(out=sidx[:, gq, 8:16], in_max=stop[:, gq, 8:16],
                                                    in_values=sq), reads=[Rsq, Rstop[gq]], writes=[Rsidx[gq]])
                    yield
                stv = stop[:].rearrange("p (h two) k -> p h two k", two=2)
                op("dve", lambda e: e.tensor_tensor(
                    out=cand[:], in0=stv[:, :, 0, :].unsqueeze(3).to_broadcast([128, 8, 16, 16]),
                    in1=stv[:, :, 1, :].unsqueeze(2).to_broadcast([128, 8, 16, 16]), op=ALU.add),
                   reads=Rstop, writes=[Rcand])
                op("act", lambda e: e.activation(out=sidxf[:], in_=sidx[:], func=AF.Copy), reads=Rsidx,
                   writes=[Rsidxf])
                yield
                for h in range(8):
                    cv = cand[:, h, :, :].rearrange("p a b -> p (a b)")
                    op("dve", lambda e: e.max(out=best[:, h, 0:8], in_=cv), reads=[Rcand], writes=[Rbest[h]])
                    yield
                    op("dve", lambda e: e.max_index(out=ci[:, h, 0:8], in_max=best[:, h, 0:8], in_values=cv),
                       reads=[Rcand, Rbest[h]], writes=[Rci[h]])
                    yield
                    op("dve", lambda e: e.match_replace(out=scr[:], in_to_replace=best[:, h, 0:8], in_values=cv,
                                                        imm_value=NEG), reads=[Rcand, Rbest[h]], writes=Rscr2)
                    yield
                    op("dve", lambda e: e.max(out=best[:, h, 8:16], in_=scr[:]), reads=Rscr2, writes=[Rbest[h]])
                    yield
                    op("dve", lambda e: e.max_index(out=ci[:, h, 8:16], in_max=best[:, h, 8:16], in_values=scr[:]),
                       reads=Rscr2 + [Rbest[h]], writes=[Rci[h]])
                    yield
                op("dve", lambda e: e.tensor_tensor(out=ge[:], in0=best[:],
                                                    in1=best[:, :, 0:1].to_broadcast([128, 8, 16]),
                                                    op=ALU.subtract), reads=Rbest, writes=[Rge])
                op("act", lambda e: e.activation(out=ge[:], in_=ge[:], func=AF.Exp), reads=[Rge], writes=[Rge])
                op("dve", lambda e: e.tensor_reduce(out=gs[:, 0:8], in_=ge[:], axis=AX.X, op=ALU.add),
                   reads=[Rge], writes=[Rgs])
                op("dve", lambda e: e.reciprocal(out=gs[:, 8:16], in_=gs[:, 0:8]), reads=[Rgs], writes=[Rgs])
                op("dve", lambda e: e.tensor_tensor(out=gate[j][:], in0=ge[:],
                                                    in1=gs[:, 8:16].unsqueeze(2).to_broadcast([128, 8, 16]),
                                                    op=ALU.mult), reads=[Rge, Rgs], writes=[Rgate[j]])
                op("dve", lambda e: e.tensor_single_scalar(out=hi[:], in_=ci[:], scalar=4,
                                                           op=ALU.logical_shift_right), reads=Rci, writes=[Rhl])
                op("dve", lambda e: e.tensor_single_scalar(out=lo[:], in_=ci[:], scalar=15, op=ALU.bitwise_and),
                   reads=Rci, writes=[Rhl])
                op("act", lambda e: e.activation(out=hif[:], in_=hi[:], func=AF.Copy), reads=[Rhl], writes=[Rhl])
                op("act", lambda e: e.activation(out=lof[:], in_=lo[:], func=AF.Copy), reads=[Rhl], writes=[Rhl])
                sxv = sidxf[:].rearrange("p (h two) k -> p h two k", two=2)
                for p_, (src, dsti) in enumerate(((hif, i1), (lof, i2))):
                    op("dve", lambda e: e.tensor_tensor(
                        out=oh[:], in0=src[:].unsqueeze(3).to_broadcast([128, 8, 16, 16]), in1=iota[:],
                        op=ALU.is_equal), reads=[Rhl, Rconst], writes=[Roh])
                    op("dve", lambda e: e.tensor_tensor(
                        out=oh[:], in0=oh[:], in1=sxv[:, :, p_, :].unsqueeze(2).to_broadcast([128, 8, 16, 16]),
                        op=ALU.mult), reads=[Roh, Rsidxf], writes=[Roh])
                    op("dve", lambda e: e.tensor_reduce(out=dsti[:], in_=oh[:], axis=AX.X, op=ALU.add),
                       reads=[Roh], writes=[Ri12])
                op("dve", lambda e: e.scalar_tensor_tensor(out=ef[:], in0=i1[:], scalar=128.0, in1=i2[:],
                                                           op0=ALU.mult, op1=ALU.add), reads=[Ri12], writes=[Ri12])
                op("dve", lambda e: e.tensor_copy(out=eidx[j][:].rearrange("p (h k) -> p h k", h=8), in_=ef[:]),
                   reads=[Ri12], writes=[Reidx[j]])
                yield

            def stage2(b, i, nxt, prev_final):
                g = b * NT + i
                j = g % 2
                jx = g % 3
                row0 = g * 128
                vb0 = 4 if g % 2 == 0 else 2
                GS = 4

                def finish(q):
                    c0_ = q * GS
                    hq = c0_ // 16
                    op("act", lambda e: e.activation(out=gl[:, c0_:c0_ + GS], in_=actv[:, c0_:c0_ + GS], func=AF.Gelu),
                       reads=Ractv[c0_:c0_ + GS], writes=[Rgl[q % 4]])
                    op("dve", lambda e: e.tensor_tensor(out=wv[:, c0_:c0_ + GS], in0=gl[:, c0_:c0_ + GS],
                                                        in1=gate[j][:, hq, c0_ - 16 * hq:c0_ - 16 * hq + GS],
                                                        op=ALU.mult), reads=[Rgl[q % 4], Rgate[j]], writes=[Rwv[q % 4]])
                    for jj in range(c0_, c0_ + GS):
                        su = (g * 128 + jj) % NUV
                        sd = jj % ND
                        op("act", lambda e: e.activation(out=dg[sd][:], in_=identb[:], func=AF.Copy,
                                                         scale=wv[:, jj:jj + 1]),
                           reads=[Rconst, Rwv[q % 4]], writes=[Rdg[sd]])
                        for hf in range(2):
                            op("pe", lambda e: e.matmul(B[vb0 + hf][:], dg[sd][:],
                                                        UV[su][:, D + hf * 512:D + (hf + 1) * 512],
                                                        start=(jj == 0), stop=(jj == 127), skip_group_check=True),
                               reads=[Rdg[sd], RUV[su]], writes=[RB[vb0 + hf]])

                NG = 128 // GS
                for q in range(NG):
                    for jj in range(q * GS, (q + 1) * GS):
                        su = (g * 128 + jj) % NUV
                        pp = (jj // 3) % 3
                        op("pool", lambda e: e.indirect_dma_start(
                            out=UV[su][:], out_offset=None, in_=uvd[:, :],
                            in_offset=bass.IndirectOffsetOnAxis(ap=eidx[j][:, jj:jj + 1], axis=0)),
                           reads=[Reidx[j]], writes=[RUV[su]], chan=cUV[su])
                        if jj % 2 == 0:
                            op("dve", lambda e: e.tensor_tensor(out=prod[pp][:], in0=UV[su][:, 0:D], in1=h2bf[j][:],
                                                                op=ALU.mult),
                               reads=[RUV[su], Rh2bf[j]], writes=[Rprod[pp]])
                            op("act", lambda e: e.activation(out=prod[pp][:], in_=prod[pp][:], func=AF.Copy,
                                                             accum_out=actv[:, jj:jj + 1]),
                               reads=[Rprod[pp]], writes=[Rprod[pp], Ractv[jj]])
                        else:
                            op("dve", lambda e: e.scalar_tensor_tensor(
                                out=UV[su][:, 0:D], in0=UV[su][:, 0:D], scalar=1.0, in1=h2bf[j][:], op0=ALU.mult,
                                op1=ALU.mult, accum_out=actv[:, jj:jj + 1]),
                               reads=[RUV[su], Rh2bf[j]], writes=[RUV[su], Ractv[jj]])
                    if q >= 2:
                        finish(q - 2)
                    if q == 2:
                        if prev_final is not None:
                            prev_final()
                        if i == 0:
                            S_.dma("sp", cm2, gt2[:], modrow[b:b + 1, 5 * D:6 * D].partition_broadcast(128),
                                   writes=[Rmod2])
                    if nxt is not None:
                        for _ in range(2 * GS):
                            next(nxt, None)
                finish(NG - 2)
                finish(NG - 1)
                if nxt is not None:
                    for _ in nxt:
                        pass

                def final():
                    for hf in range(2):
                        op("dve", lambda e: e.tensor_tensor(out=tmp2[:, hf * 512:(hf + 1) * 512], in0=B[vb0 + hf][:],
                                                            in1=gt2[:, hf * 512:(hf + 1) * 512], op=ALU.mult),
                           reads=[RB[vb0 + hf], Rmod2], writes=[Rtmp2])
                    op("dve", lambda e: e.tensor_tensor(out=xb[jx][:], in0=xb[jx][:], in1=tmp2[:], op=ALU.add),
                       reads=[Rtmp2, Rxb[jx]], writes=[Rxb[jx]])
                    op("act", lambda e: e.activation(out=tmp2[:], in_=xb[jx][:], func=AF.Square, scale=1.0 / 32,
                                                     accum_out=st2[:, 4:5]), reads=[Rxb[jx]], writes=[Rtmp2, Rst2])
                    op("act", lambda e: e.activation(out=st2[:, 5:6], in_=st2[:, 4:5], func=AF.Sqrt,
                                                     bias=cst[:, 0:1]),
                       reads=[Rst2, Rconst], writes=[Rst2])
                    op("dve", lambda e: e.reciprocal(out=st2[:, 6:7], in_=st2[:, 5:6]), reads=[Rst2], writes=[Rst2])
                    op("dve", lambda e: e.scalar_tensor_tensor(out=xb[jx][:], in0=xb[jx][:], scalar=st2[:, 6:7],
                                                               in1=fgb[:], op0=ALU.mult, op1=ALU.mult),
                       reads=[Rxb[jx], Rst2, Rconst], writes=[Rxb[jx]])
                    S_.dma("sp", cxs[jx], out[row0:row0 + 128, :], xb[jx][:], reads=[Rxb[jx]])
                return final

            tiles = [(b, i) for b in range(NB) for i in range(NT)]
            for _ in stage1(*tiles[0]):
                pass
            pf = None
            for n_, (b, i) in enumerate(tiles):
                nxt = stage1(*tiles[n_ + 1]) if n_ + 1 < len(tiles) else None
                pf = stage2(b, i, nxt, pf)
            pf()
            S_.barrier(bres)
            S_.emit()
    return nc


def _host_consts():
    c = np.zeros((128, 6, 128), np.float32)
    s = np.arange(128)[:, None]
    l = np.arange(128)[None, :]
    same = (s // 64) == (l // 64)
    c[:, 0, :] = np.eye(128, dtype=np.float32)
    c[:, 1, :] = (same & (s <= l)).astype(np.float32)
    c[:, 2, :] = (same & (s > l)).astype(np.float32)
    c[:, 3, :] = np.broadcast_to((s < 64), (128, 128)).astype(np.float32)
    c[:, 4, :] = np.broadcast_to((s >= 64), (128, 128)).astype(np.float32)
    return c


def prep_shared(inp):
    f = lambda a: np.ascontiguousarray(np.asarray(a, dtype=np.float32))
    rb = f(inp["rel_bias"])[0]
    k = np.arange(128)[:, None]
    l = np.arange(128)[None, :]
    eb = np.zeros((128, 4, 8, 128), np.float32)
    for rc, r in enumerate((0, 1, 2, 4)):
        idx = np.clip(128 * r + l - k, -128, 128) + 128
        eb[:, rc, :, :] = np.transpose(rb[:, idx], (1, 0, 2))
    d = {
        "w_ada": f(inp["w_ada"])[0],
        "b_ada": f(inp["b_ada"]),
        "norm1_g": f(inp["norm1_g"]),
        "w_in": f(inp["w_in"])[0],
        "cwT": f(f(inp["conv_w"])[0].T.reshape(8, 128, 4).transpose(1, 0, 2)),
        "cbT": f(f(inp["conv_b"])[0].reshape(8, 128).T),
        "bif": f(np.concatenate([f(inp["b_igate"]), f(inp["b_fgate"])], axis=1)),
        "ebsrc": eb,
        "gmn": f(inp["mlstm_norm_g"]),
        "w_branch_a": f(inp["w_branch_a"])[0],
        "w_branch_b": f(inp["w_branch_b"])[0],
        "w_out": f(inp["w_out"])[0],
        "norm2_g": f(inp["norm2_g"]),
        "w_peer_q": f(inp["w_peer_q"])[0],
        "keysT": f(f(inp["peer_sub_keys"])[0].reshape(16, 128, 128).transpose(2, 0, 1)),
        "peer_u": f(inp["peer_u"])[0],
        "peer_v": f(inp["peer_v"])[0],
        "final_g": f(inp["final_g"]).reshape(1, D),
        "consts": _host_consts(),
        "iota16": np.ascontiguousarray(np.broadcast_to((np.arange(2048) % 16).astype(np.float32), (128, 2048))),
    }
    return d


def prep_core(inp, b0, NB):
    f = lambda a: np.ascontiguousarray(np.asarray(a, dtype=np.float32))
    xs = f(inp["x"][b0:b0 + NB]).reshape(-1, D)
    c = f(inp["c"][b0:b0 + NB])
    cT = f(c.reshape(NB, 8, 128).transpose(2, 0, 1))
    return {"x": xs, "cT": cT}


_NC_CACHE = {}


def kernel(**inputs):
    n = 8
    NB = 2
    S = 4096
    key = (NB, S)
    if key not in _NC_CACHE:
        _NC_CACHE[key] = build(NB, S)
    nc = _NC_CACHE[key]
    shared = prep_shared(inputs)
    in_maps = []
    for i in range(n):
        m = dict(shared)
        m.update(prep_core(inputs, i * NB, NB))
        in_maps.append(m)
    res = run_bass_kernel_spmd(nc, in_maps, core_ids=list(range(n)))
    outs = [np.asarray(r["out"]).reshape(NB, S, D) for r in res.results]
    return np.concatenate(outs, axis=0).astype(np.float32)
```

```python
import numpy as np
from contextlib import ExitStack
import concourse.bass as bass
import concourse.mybir as mybir
from concourse.bass_utils import run_bass_kernel_spmd

F32 = mybir.dt.float32
BF16 = mybir.dt.bfloat16
U32 = mybir.dt.uint32
I32 = mybir.dt.int32
ALU = mybir.AluOpType
AF = mybir.ActivationFunctionType
AX = mybir.AxisListType

D = 1024
IN_COLS = 5640
EPS = 1e-6
NEG = -1.0e30
ENGS = ("pe", "act", "dve", "pool", "sp")


class Res:
    __slots__ = ("name", "w", "r", "excl")

    def __init__(self, name, excl=False):
        self.name = name
        self.w = None
        self.r = []
        self.excl = excl


class Chan:
    def __init__(self, sem, name):
        self.sem = sem
        self.name = name
        self.n = 0
        self.last = None


class _Rec:
    def __getattr__(self, name):
        return lambda *a, **k: (name, a, k)


_REC = _Rec()


class Sched:
    def __init__(self, nc, es):
        self.nc = nc
        self.es = es
        self.ops = []
        self.chans = []
        self.sems = {e: es.enter_context(nc.semaphore("eng_" + e)) for e in ENGS}
        self.emitted = 0
        self.cnt = {e: 0 for e in ENGS}
        self.known = {e: {} for e in ENGS}
        self.prog = {}
        self.clock = {}

    def chan(self, name):
        c = Chan(self.es.enter_context(self.nc.semaphore("ch_" + name)), name)
        self.chans.append(c)
        return c

    limit = None
    in_bar = False

    def op(self, eng, fn, reads=(), writes=(), chan=None, extra=()):
        oid = len(self.ops)
        if self.limit is not None and oid >= self.limit and not self.in_bar:
            return None
        deps = set(extra)
        for r in reads:
            if r.w is not None:
                deps.add(r.w)
            if r.excl:
                deps.update(r.r)
        for w in writes:
            if w.w is not None:
                deps.add(w.w)
            deps.update(w.r)
        if chan is not None and chan.last is not None:
            deps.add(chan.last)
        for r in reads:
            if r.excl:
                r.w = oid
                r.r = []
            else:
                r.r.append(oid)
        for w in writes:
            w.w = oid
            w.r = []
        if chan is not None:
            chan.last = oid
        deps.discard(oid)
        name, a, k = fn(_REC)
        fn = (lambda e, name=name, a=a, k=k: getattr(e, name)(*a, **k))
        self.ops.append((eng, fn, deps, chan))
        return oid

    def dma(self, eng, chan, out, in_, reads=(), writes=()):
        return self.op(eng, lambda e: e.dma_start(out=out, in_=in_), reads, writes, chan=chan)

    def barrier(self, bres):
        dum, dps, dd, cbar = self.bar_objs
        self.in_bar = True
        fns = {
            "pe": lambda g: g.matmul(dps[0:2, 0:2], dum[:, 0:2], dum[:, 2:4], start=True, stop=True,
                                     skip_group_check=True),
            "act": lambda g: g.activation(out=dum[:, 4:5], in_=dum[:, 0:1], func=AF.Copy),
            "dve": lambda g: g.tensor_copy(out=dum[:, 5:6], in_=dum[:, 0:1]),
            "pool": lambda g: g.tensor_copy(out=dum[:, 6:7], in_=dum[:, 0:1]),
        }
        for rnd in range(2):
            for e in ENGS:
                rd = ([bres[f] for f in ENGS] if rnd == 1 else []) + [bres["dum"]]
                if e == "sp":
                    ex = [c.last for c in self.chans if c.last is not None]
                    self.op("sp", lambda g, rnd=rnd: g.dma_start(out=dd[rnd:rnd + 1, :], in_=self.bar_src), reads=rd, writes=[bres[e]],
                            chan=cbar, extra=ex)
                else:
                    self.op(e, fns[e], reads=rd + ([bres["pe_bank"]] if e == "pe" else []), writes=[bres[e]])
        self.in_bar = False

    def emit(self):
        ops = self.ops
        lo = self.emitted
        hi = len(ops)
        needed = set()
        for i in range(lo, hi):
            eng, fn, deps, chan = ops[i]
            for d in deps:
                de, _, _, dc = ops[d]
                if de == "pe" and eng == "pe" and dc is None and chan is None:
                    continue
                if d >= lo:
                    needed.add(d)
        per_eng = {e: [] for e in ENGS}
        for i in range(lo, hi):
            eng, fn, deps, chan = ops[i]
            waits = []
            kn = self.known[eng]
            for d in sorted(deps):
                de, _, _, dc = ops[d]
                if de == "pe" and eng == "pe" and dc is None and chan is None:
                    continue
                if d < lo:
                    continue
                key, val = self.prog[d]
                if kn.get(key, 0) >= val:
                    continue
                waits.append((key, val))
                kn[key] = val
                ck = self.clock.get(d)
                if ck is not None:
                    for k2, v2 in ck.items():
                        if kn.get(k2, 0) < v2:
                            kn[k2] = v2
            wmax = {}
            for key, val in waits:
                if wmax.get(key, 0) < val:
                    wmax[key] = val
            inc = None
            if chan is not None:
                chan.n += 1
                self.prog[i] = (chan, chan.n)
                self.clock[i] = {k: v for k, v in kn.items() if isinstance(k, str)}
                inc = (chan.sem, 16)
            elif i in needed:
                self.cnt[eng] += 1
                self.prog[i] = (eng, self.cnt[eng])
                ck = {k: v for k, v in kn.items() if isinstance(k, str)}
                ck[eng] = self.cnt[eng]
                self.clock[i] = ck
                inc = (self.sems[eng], 1)
            per_eng[eng].append((list(wmax.items()), fn, inc))
        self.emitted = hi
        sems = self.sems

        def replay(e, lst):
            for waits, fn, inc in lst:
                for key, val in waits:
                    if isinstance(key, str):
                        e.wait_ge(sems[key], val)
                    else:
                        e.wait_ge(key.sem, val * 16)
                ins = fn(e)
                if inc is not None:
                    ins.then_inc(inc[0], inc[1])

        with self.nc.Block() as block:
            @block.tensor
            def _(e):
                replay(e, per_eng["pe"])

            @block.scalar
            def _(e):
                replay(e, per_eng["act"])

            @block.vector
            def _(e):
                replay(e, per_eng["dve"])

            @block.gpsimd
            def _(e):
                replay(e, per_eng["pool"])

            @block.sync
            def _(e):
                replay(e, per_eng["sp"])


def build(NB, S, debug=False, do_b=True):
    T = NB * S
    NT = S // 128
    nc = bass.Bass("TRN2", target_bir_lowering=False)

    def din(name, shape, dt=F32):
        return nc.dram_tensor(name, list(shape), dt, kind="ExternalInput").ap()

    x = din("x", [T, D])
    cT = din("cT", [128, NB, 8])
    w_ada = din("w_ada", [D, 6 * D])
    b_ada = din("b_ada", [1, 6 * D])
    g1 = din("norm1_g", [1, D])
    w_in = din("w_in", [D, IN_COLS])
    cwT = din("cwT", [128, 8, 4])
    cbT = din("cbT", [128, 8])
    bif = din("bif", [1, 8])
    ebsrc = din("ebsrc", [128, 4, 8, 128])
    gmn_d = din("gmn", [1, 512])
    wba_d = din("w_branch_a", [512, D])
    wbb_d = din("w_branch_b", [512, D])
    wout_d = din("w_out", [D, D])
    g2 = din("norm2_g", [1, D])
    wpq_d = din("w_peer_q", [D, 2048])
    keysT = din("keysT", [128, 16, 128])
    pu = din("peer_u", [16384, D])
    pv = din("peer_v", [16384, D])
    fg = din("final_g", [1, D])
    consts_d = din("consts", [128, 6, 128])
    iota_d = din("iota16", [128, 2048])
    out = nc.dram_tensor("out", [T, D], F32, kind="ExternalOutput").ap()
    modrow = nc.dram_tensor("modrow", [NB, 6 * D], F32, kind="Internal").ap()
    x1d = nc.dram_tensor("x1d", [T, D], F32, kind="Internal").ap()
    uvd = nc.dram_tensor("uvd", [16384, 2 * D], BF16, kind="Internal").ap()
    dbg = None
    if debug:
        dbg = nc.dram_tensor("dbg", [T, D], F32, kind="ExternalOutput").ap()

    es = ExitStack()
    with es:
        S_ = Sched(nc, es)
        op = S_.op
        bres = {e: Res("bar_" + e) for e in ENGS}
        dum = es.enter_context(nc.sbuf_tensor("bar_dum", [128, 8], F32))
        ddr = nc.dram_tensor("bar_dd", [2, 16], F32, kind="Internal").ap()
        ps = ExitStack()
        es.enter_context(ps)
        B = [ps.enter_context(nc.psum_tensor("pb%d" % i, [128, 512], F32)) for i in range(6)]
        Tb = [ps.enter_context(nc.psum_tensor("pt%d" % i, [128, 1024], BF16)) for i in range(2)]
        RB = [Res("B%d" % i, excl=True) for i in range(6)]
        RT = [Res("T%d" % i, excl=True) for i in range(2)]
        bres["pe_bank"] = RB[5]
        S_.bar_objs = (dum, B[5], ddr, S_.chan("bar"))
        S_.bar_src = b_ada[0:1, 0:16]
        bres["dum"] = Res("dum")
        op("pool", lambda e: e.memset(dum[:], 0.0), writes=[bres["dum"]])

        with ExitStack() as p0:
            def sb(name, shape, dt=F32):
                return p0.enter_context(nc.sbuf_tensor("p0_" + name, list(shape), dt))
            stg = [sb("p0stg%d" % i, [128, 1024]) for i in range(3)]
            Rstg = [Res("p0stg%d" % i) for i in range(3)]
            cstg = [S_.chan("p0stg%d" % i) for i in range(3)]
            scb = sb("scb", [128, NB, 8])
            Rscb = Res("scb")
            brow = sb("brow", [1, 6 * D])
            Rbrow = Res("brow")
            mrow = sb("mrow", [1, NB, 6 * D])
            Rmrow = Res("mrow")
            c0 = S_.chan("p0a")
            S_.dma("sp", c0, scb[:], cT[:, :, :], writes=[Rscb])
            S_.dma("sp", c0, brow[:], b_ada[:, :], writes=[Rbrow])
            op("act", lambda e: e.activation(out=scb[:], in_=scb[:], func=AF.Silu), reads=[Rscb], writes=[Rscb])
            li = 0
            for cg in range(6):
                for k in range(8):
                    j = li % 3
                    li += 1
                    S_.dma("sp", cstg[j], stg[j][:], w_ada[k * 128:(k + 1) * 128, cg * 1024:(cg + 1) * 1024],
                           writes=[Rstg[j]])
                    for b in range(NB):
                        for hf in range(2):
                            bk = b * 2 + hf
                            op("pe", lambda e, bk=bk, b=b, k=k, j=j, hf=hf: e.matmul(
                                B[bk][0:1, :], scb[:, b, k:k + 1], stg[j][:, hf * 512:(hf + 1) * 512],
                                start=(k == 0), stop=(k == 7), skip_group_check=True),
                               reads=[Rscb, Rstg[j]], writes=[RB[bk]])
                for b in range(NB):
                    for hf in range(2):
                        bk = b * 2 + hf
                        c0_ = cg * 1024 + hf * 512
                        op("dve", lambda e, bk=bk, b=b, c0_=c0_: e.tensor_tensor(
                            out=mrow[0:1, b, c0_:c0_ + 512], in0=B[bk][0:1, :], in1=brow[0:1, c0_:c0_ + 512],
                            op=ALU.add), reads=[RB[bk], Rbrow], writes=[Rmrow])
            S_.dma("sp", c0, modrow.rearrange("(o b) n -> o b n", o=1), mrow[:], reads=[Rmrow])
            NCV = 6
            cvin = [sb("cvin%d" % i, [128, 2 * D]) for i in range(NCV)]
            cvout = [sb("cvout%d" % i, [128, 2 * D], BF16) for i in range(NCV)]
            Rcvin = [Res("cvin%d" % i) for i in range(NCV)]
            Rcvout = [Res("cvout%d" % i) for i in range(NCV)]
            ccvu = [S_.chan("cvu%d" % i) for i in range(NCV)]
            ccvv = [S_.chan("cvv%d" % i) for i in range(NCV)]
            ccvs = [S_.chan("cvs%d" % i) for i in range(NCV)]
            def cv_load(r):
                k = r % NCV
                S_.dma("sp", ccvu[k], cvin[k][:, 0:D], pu[r * 128:(r + 1) * 128, :], writes=[Rcvin[k]])
                S_.dma("sp", ccvv[k], cvin[k][:, D:2 * D], pv[r * 128:(r + 1) * 128, :], writes=[Rcvin[k]])
            for r in range(NCV - 1):
                cv_load(r)
            for r in range(128):
                k = r % NCV
                if r + NCV - 1 < 128:
                    cv_load(r + NCV - 1)
                eng = ("dve", "act", "dve", "dve", "act", "pool")[r % 6]
                if eng == "act":
                    op("act", lambda e: e.activation(out=cvout[k][:], in_=cvin[k][:], func=AF.Copy),
                       reads=[Rcvin[k]], writes=[Rcvout[k]])
                else:
                    op(eng, lambda e: e.tensor_copy(out=cvout[k][:], in_=cvin[k][:]), reads=[Rcvin[k]],
                       writes=[Rcvout[k]])
                S_.dma("act", ccvs[k], uvd[r * 128:(r + 1) * 128, :], cvout[k][:], reads=[Rcvout[k]])
            S_.barrier(bres)
            S_.emit()

        with ExitStack() as pa:
            tot = [0]

            def sb(name, shape, dt=F32):
                n = int(np.prod(shape[1:])) * (4 if dt in (F32, U32, I32) else 2)
                tot[0] += n
                return pa.enter_context(nc.sbuf_tensor("a_" + name, list(shape), dt))
            NXB = 2
            win = sb("win", [128, 8, IN_COLS], BF16)
            wba = sb("wba", [128, 4, D], BF16)
            wbb = sb("wbb", [128, 4, D], BF16)
            wout = sb("wout", [128, 8, D], BF16)
            consts = sb("consts", [128, 6, 128])
            identb = sb("identb", [128, 128], BF16)
            EB = sb("EB", [128, 4, 8, 128], BF16)
            A1 = sb("A1", [128, D])
            sh1 = sb("sh1", [128, D])
            gt1 = sb("gt1", [128, D])
            gmn = sb("gmn", [128, 512])
            cw = sb("cw", [128, 8, 4])
            cb = sb("cb", [128, 8])
            bifb = sb("bifb", [128, 8])
            cst = sb("cst", [128, 4])
            nh4 = sb("nh4", [128, 4])
            xb = [sb("xb%d" % i, [128, D]) for i in range(NXB)]
            tmp = sb("tmp", [128, D])
            hT = sb("hT", [128, 8, 128], BF16)
            qTz = sb("qTz", [128, 4, 2, 128], BF16)
            kTh = sb("kTh", [128, 4, 5, 128], BF16)
            vh = sb("vh", [128, 5, 8, 65], BF16)
            ub = sb("ub", [128, 8, 131])
            cacc = [sb("cacc%d" % i, [128, 128]) for i in range(2)]
            qbT = sb("qbT", [128, 4, 128], BF16)
            kbT = sb("kbT", [128, 4, 128], BF16)
            zq = sb("zq", [128, 4, 2, 128], BF16)
            sga = sb("sga", [128, 8, 128], BF16)
            sgb = sb("sgb", [128, 8, 128], BF16)
            vba = sb("vba", [128, 4, 129], BF16)
            sob = sb("sob", [128, 512])
            gsb = sob
            gsm = sb("gsm", [128, 8])
            PTa = [sb("PTa%d" % i, [128, 5, 2, 128], BF16) for i in range(2)]
            ya = sb("ya", [128, 512], BF16)
            yaT = sb("yaT", [128, 4, 128], BF16)
            yb = sb("yb", [128, 512], BF16)
            ybT = sb("ybT", [128, 4, 128], BF16)
            Cst = sb("Cst", [128, 4, 129])
            Cbf = [sb("Cbf%d" % i, [128, 4, 129], BF16) for i in range(2)]
            kwz = sb("kwz", [128, 2, 4, 128], BF16)
            PTm = sb("PTm", [128, 4, 128], BF16)
            yT = sb("yT", [128, 8, 128], BF16)
            st = sb("st", [128, 64])
            print("phaseA sbuf bytes/partition:", tot[0])

            Rw = Res("weights")
            Rconst = Res("consts")
            Rmod = Res("mod")
            Rxb = [Res("xb%d" % i) for i in range(NXB)]
            cxb = [S_.chan("xb%d" % i) for i in range(NXB)]
            cxs = [S_.chan("xs%d" % i) for i in range(NXB)]
            cmisc = S_.chan("misc")
            cmisc2 = S_.chan("misc2")
            Rtmp = Res("tmp"); RhT = Res("hT"); RqT = Res("qT")
            RkTh = [Res("kTh%d" % i) for i in range(5)]
            Rvh = [Res("vh%d" % i) for i in range(5)]
            Rub = Res("ub"); Rcacc = [Res("cacc0"), Res("cacc1")]
            RqbT = Res("qbT"); RkbT = Res("kbT"); Rzq = Res("zq"); Rsga = Res("sga"); Rsgb = Res("sgb")
            Rvba = Res("vba"); Rsob = Res("sob"); Rgsb = Rsob; Rgsm = Res("gsm")
            RPTa = [Res("PTa0"), Res("PTa1")]
            Rya = Res("ya"); RyaT = Res("yaT"); Ryb = Res("yb"); RybT = Res("ybT")
            RC = Res("Cst"); RCbf = [Res("Cbf0"), Res("Cbf1")]
            Rkw = Res("kw"); RPTm = Res("PTm"); RyT = Res("yT")
            Rst = Res("st"); Rst2 = Res("st2"); Rst3 = Res("st3")
            hbf = yT[:].rearrange("p k t -> p (k t)")
            Rhbf = RyT
            hb = tmp[:, 512:1024].rearrange("p (h d) -> p h d", h=4)
            Rhb = Rtmp

            S_.dma("sp", cmisc, consts[:], consts_d[:, :, :], writes=[Rconst])
            S_.dma("sp", cmisc, cw[:], cwT[:, :, :], writes=[Rconst])
            S_.dma("sp", cmisc, cb[:], cbT[:, :], writes=[Rconst])
            S_.dma("sp", cmisc, bifb[:], bif[0:1, :].partition_broadcast(128), writes=[Rconst])
            S_.dma("sp", cmisc, gmn[:], gmn_d[0:1, :].partition_broadcast(128), writes=[Rconst])
            op("dve", lambda e: e.tensor_copy(out=identb[:], in_=consts[:, 0, :]), reads=[Rconst], writes=[Rconst])
            op("pool", lambda e: e.memset(cst[:, 0:1], EPS), writes=[Rconst])
            op("pool", lambda e: e.memset(nh4[:], -0.5), writes=[Rconst])
            op("pool", lambda e: e.memset(cst[:, 1:2], 1.0), writes=[Rconst])
            op("pool", lambda e: e.memset(cst[:, 2:3], float(np.log(128.0 ** -0.5))), writes=[Rconst])
            op("pool", lambda e: e.memset(vh[:], 1.0), writes=Rvh)
            op("pool", lambda e: e.memset(vba[:], 1.0), writes=[Rvba])
            op("pool", lambda e: e.memset(zq[:], 0.0), writes=[Rzq])
            op("pool", lambda e: e.memset(qTz[:], 0.0), writes=[RqT])
            op("pool", lambda e: e.memset(kwz[:], 0.0), writes=[Rkw])
            cvt_i = [0]

            def load_cast(dst, src, ncols):
                j = cvt_i[0] % NXB
                engs = ("dve", "act", "pool")
                eng = engs[cvt_i[0] % 3]
                cvt_i[0] += 1
                S_.dma("sp", cxb[j], xb[j][:, 0:ncols], src, writes=[Rxb[j]])
                if eng == "act":
                    op("act", lambda e: e.activation(out=dst, in_=xb[j][:, 0:ncols], func=AF.Copy),
                       reads=[Rxb[j]], writes=[Rw])
                else:
                    op(eng, lambda e: e.tensor_copy(out=dst, in_=xb[j][:, 0:ncols]), reads=[Rxb[j]], writes=[Rw])
            for k in range(8):
                for c0_ in range(0, IN_COLS, 1024):
                    n = min(1024, IN_COLS - c0_)
                    load_cast(win[:, k, c0_:c0_ + n], w_in[k * 128:(k + 1) * 128, c0_:c0_ + n], n)
            for k in range(4):
                load_cast(wba[:, k, :], wba_d[k * 128:(k + 1) * 128, :], 1024)
                load_cast(wbb[:, k, :], wbb_d[k * 128:(k + 1) * 128, :], 1024)
            for k in range(8):
                load_cast(wout[:, k, :], wout_d[k * 128:(k + 1) * 128, :], 1024)
            for rc in range(4):
                j = cvt_i[0] % NXB
                cvt_i[0] += 1
                S_.dma("sp", cxb[j], xb[j][:], ebsrc[:, rc, :, :].rearrange("p h l -> p (h l)"), writes=[Rxb[j]])
                op("act", lambda e, j=j, rc=rc: e.activation(
                    out=EB[:, rc, :, :].rearrange("p h l -> p (h l)"), in_=xb[j][:], func=AF.Exp),
                   reads=[Rxb[j]], writes=[Rconst])
            op("pool", lambda e: e.memset(EB[64:128, 0, :, 0:64], 0.0), writes=[Rconst])
            op("pool", lambda e: e.memset(EB[0:64, 3, :, 64:128], 0.0), writes=[Rconst])

            def slot(i):
                return i % 5

            cpar_box = [0]
            RmodG = Res('modG')

            def sa1(b, i):
                g = b * NT + i
                j = g % NXB
                row0 = g * 128
                sl = slot(i)
                if i == 0:
                    S_.dma('sp', cmisc, A1[:], g1[0:1, :].partition_broadcast(128), writes=[Rmod])
                    S_.dma('sp', cmisc, tmp[:], modrow[b:b + 1, D:2 * D].partition_broadcast(128), writes=[Rtmp])
                    S_.dma('sp', cmisc, sh1[:], modrow[b:b + 1, 0:D].partition_broadcast(128), writes=[Rmod])
                    op('dve', lambda e: e.scalar_tensor_tensor(out=A1[:], in0=tmp[:], scalar=1.0, in1=A1[:],
                                                               op0=ALU.add, op1=ALU.mult),
                       reads=[Rtmp, Rmod], writes=[Rmod])
                    op('pool', lambda e: e.memset(ub[:, :, 0:3], 0.0), writes=[Rub])
                if g == 0:
                    S_.dma("sp", cxb[j], xb[j][:], x[row0:row0 + 128, :], writes=[Rxb[j]])
                op("act", lambda e, j=j: e.activation(out=tmp[:], in_=xb[j][:], func=AF.Square, scale=1.0 / 32,
                                                      accum_out=st[:, 0:1]), reads=[Rxb[j]], writes=[Rtmp, Rst])
                op("pool", lambda e: e.tensor_scalar(out=st[:, 1:2], in0=st[:, 0:1], scalar1=EPS, scalar2=None,
                                                     op0=ALU.add), reads=[Rst], writes=[Rst])
                op("pool", lambda e: e.tensor_tensor(out=st[:, 2:3], in0=st[:, 1:2], in1=nh4[:, 0:1], op=ALU.pow),
                   reads=[Rst, Rconst], writes=[Rst])
                op("dve", lambda e, j=j: e.scalar_tensor_tensor(out=tmp[:], in0=xb[j][:], scalar=st[:, 2:3],
                                                                in1=A1[:], op0=ALU.mult, op1=ALU.mult),
                   reads=[Rxb[j], Rst, Rmod], writes=[Rtmp])
                op("dve", lambda e: e.tensor_tensor(out=hbf, in0=tmp[:], in1=sh1[:], op=ALU.add),
                   reads=[Rtmp, Rmod], writes=[Rhbf])
                for k in range(8):
                    op("pe", lambda e, k=k: e.transpose(out=Tb[0][:, k * 128:(k + 1) * 128],
                                                        in_=hbf[:, k * 128:(k + 1) * 128], identity=identb[:]),
                       reads=[Rhbf, Rconst], writes=[RT[0]])
                op("act", lambda e: e.activation(out=hT[:].rearrange("p k t -> p (k t)"), in_=Tb[0][:],
                                                 func=AF.Copy), reads=[RT[0]], writes=[RhT])

                yield 'p1'
                pbank = [0]

                def fm_group(col0, evac):
                    bk = pbank[0] % 2
                    pbank[0] += 1
                    for m in range(4):
                        for k in range(8):
                            op("pe", lambda e, bk=bk, m=m, k=k: e.matmul(
                                B[bk][:, m * 128:(m + 1) * 128],
                                win[:, k, col0 + m * 128:col0 + (m + 1) * 128], hT[:, k, :],
                                start=(k == 0), stop=(k == 7), skip_group_check=True),
                               reads=[Rw, RhT], writes=[RB[bk]])
                    evac(bk)

                def tm_group(col0, ncol, evac):
                    bk = pbank[0] % 2
                    pbank[0] += 1
                    for k in range(8):
                        op("pe", lambda e, bk=bk, k=k: e.matmul(
                            B[bk][:, 0:ncol], hT[:, k, :], win[:, k, col0:col0 + ncol],
                            start=(k == 0), stop=(k == 7), skip_group_check=True),
                           reads=[Rw, RhT], writes=[RB[bk]])
                    evac(bk)

                def evac_q(bk):
                    for e_ in range(2):
                        op("act", lambda e: e.activation(
                            out=qTz[64 * e_:64 * e_ + 64, :, e_, :],
                            in_=B[bk][64 * e_:64 * e_ + 64, :].rearrange("p (m t) -> p m t", m=4),
                            func=AF.Copy, scale=0.125), reads=[RB[bk]], writes=[RqT])
                fm_group(0, evac_q)
                yield 'p1'
                fm_group(512, lambda bk: op("dve", lambda e: e.tensor_copy(
                    out=kTh[:, :, sl, :], in_=B[bk][:].rearrange("p (m t) -> p m t", m=4)),
                    reads=[RB[bk]], writes=[RkTh[sl]]))
                yield 'p1'
                tm_group(1024, 512, lambda bk: op("dve", lambda e: e.tensor_copy(
                    out=vh[:, sl, :, 0:64], in_=B[bk][:].rearrange("p (h d) -> p h d", h=8)),
                    reads=[RB[bk]], writes=[Rvh[sl]]))
                yield 'P1END'
                fm_group(1536, lambda bk: op("act", lambda e: e.activation(
                    out=ub[:, 0:4, 3:131], in_=B[bk][:].rearrange("p (m t) -> p m t", m=4), func=AF.Copy),
                    reads=[RB[bk]], writes=[Rub]))
                yield 'p2'
                fm_group(2048, lambda bk: op("dve", lambda e: e.tensor_copy(
                    out=ub[:, 4:8, 3:131], in_=B[bk][:].rearrange("p (m t) -> p m t", m=4)),
                    reads=[RB[bk]], writes=[Rub]))
                yield 'p2'
                tm_group(2560, 512, lambda bk: op("dve", lambda e: e.tensor_copy(
                    out=vba[:, :, 0:128], in_=B[bk][:].rearrange("p (h d) -> p h d", h=4)),
                    reads=[RB[bk]], writes=[Rvba]))
                yield 'p2'
                tm_group(3072, 512, lambda bk: op("act", lambda e: e.activation(
                    out=sob[:], in_=B[bk][:], func=AF.Sigmoid), reads=[RB[bk]], writes=[Rsob]))
                op("pool", lambda e: e.tensor_tensor(out=gsb[:], in0=sob[:], in1=gmn[:], op=ALU.mult),
                   reads=[Rsob, Rconst], writes=[Rsob])
                yield 'p2'
                tm_group(3584, 8, lambda bk: op("dve", lambda e: e.tensor_tensor(
                    out=gsm[:], in0=B[bk][:, 0:8], in1=bifb[:], op=ALU.add),
                    reads=[RB[bk], Rconst], writes=[Rgsm]))
                yield 'p2'
                yield 'P2END'
                for q in range(2):
                    fm_group(3592 + q * 512, lambda bk, q=q: op("act", lambda e: e.activation(
                        out=sga[:, 4 * q:4 * q + 4, :].rearrange("p m t -> p (m t)"), in_=B[bk][:],
                        func=AF.Sigmoid), reads=[RB[bk]], writes=[Rsga]))
                    yield 'p3'
                for q in range(2):
                    fm_group(4616 + q * 512, lambda bk, q=q: op("act", lambda e: e.activation(
                        out=sgb[:, 4 * q:4 * q + 4, :].rearrange("p m t -> p (m t)"), in_=B[bk][:],
                        func=AF.Sigmoid), reads=[RB[bk]], writes=[Rsgb]))
                    yield 'p3'
                for m in range(8):
                    ca = cacc[m % 2]
                    Rca = Rcacc[m % 2]
                    op("dve", lambda e, m=m, ca=ca: e.tensor_scalar(
                        out=ca[:], in0=ub[:, m, 3:131], scalar1=cw[:, m, 3:4], scalar2=cb[:, m:m + 1],
                        op0=ALU.mult, op1=ALU.add), reads=[Rub, Rconst], writes=[Rca])
                    for tp in range(3):
                        op("dve", lambda e, m=m, ca=ca, tp=tp: e.scalar_tensor_tensor(
                            out=ca[:], in0=ub[:, m, tp:tp + 128], scalar=cw[:, m, tp:tp + 1], in1=ca[:],
                            op0=ALU.mult, op1=ALU.add), reads=[Rub, Rconst, Rca], writes=[Rca])
                    if m < 4:
                        op("act", lambda e, m=m, ca=ca: e.activation(out=qbT[:, m, :], in_=ca[:], func=AF.Silu),
                           reads=[Rca], writes=[RqbT])
                    else:
                        op("act", lambda e, m=m, ca=ca: e.activation(out=kbT[:, m - 4, :], in_=ca[:],
                                                                     func=AF.Silu),
                           reads=[Rca], writes=[RkbT])
                    yield 'p3'
                op("pool", lambda e: e.tensor_copy(out=ub[:, :, 0:3], in_=ub[:, :, 128:131]),
                   reads=[Rub], writes=[Rub])
                op("pool", lambda e: e.tensor_copy(out=zq[:, :, 0, 0:64], in_=qbT[:, :, 0:64]),
                   reads=[RqbT], writes=[Rzq])
                op("pool", lambda e: e.tensor_copy(out=zq[:, :, 1, 64:128], in_=qbT[:, :, 64:128]),
                   reads=[RqbT], writes=[Rzq])
                yield 'p3'

            def sa2(b, i):
                g = b * NT + i
                j = g % NXB
                row0 = g * 128
                if i == 0:
                    S_.dma('sp', cmisc2, gt1[:], modrow[b:b + 1, 2 * D:3 * D].partition_broadcast(128), writes=[RmodG])
                    op('pool', lambda e: e.memset(Cst[:], 0.0), writes=[RC])
                    op('pool', lambda e: e.memset(Cbf[0][:], 0.0), writes=[RCbf[0]])
                    cpar_box[0] = 0
                cpar = cpar_box[0]
                if g + 1 < NB * NT:
                    jn = (g + 1) % NXB
                    S_.dma("sp", cxb[jn], xb[jn][:], x[row0 + 128:row0 + 256, :], writes=[Rxb[jn]])
                nr = min(i, 4) + 1
                rcs = (0, 1, 2, 2, 3)
                def st_part(hp):
                    pt = PTa[hp % 2]
                    Rpt = RPTa[hp % 2]
                    bk0 = 2 if hp % 2 == 0 else 0
                    for r0 in range(0, nr, 2):
                        bk = bk0 + (r0 // 2) % 2
                        rr = [r for r in (r0, r0 + 1) if r < nr]
                        for r in rr:
                            ks = slot(i - r)
                            for e_ in range(2):
                                op("pe", lambda e, bk=bk, r=r, r0=r0, ks=ks, e_=e_, hp=hp: e.matmul(
                                    B[bk][:, (r - r0) * 256 + e_ * 128:(r - r0) * 256 + (e_ + 1) * 128],
                                    kTh[:, hp, ks, :], qTz[:, hp, e_, :],
                                    start=True, stop=True, skip_group_check=True),
                                   reads=[RkTh[ks], RqT], writes=[RB[bk]])
                        nn = len(rr)
                        op("act", lambda e, bk=bk, r0=r0, nn=nn, pt=pt: e.activation(
                            out=pt[:, r0:r0 + nn, :, :].rearrange("p r e t -> p (r e t)"),
                            in_=B[bk][:, 0:nn * 256], func=AF.Exp), reads=[RB[bk]], writes=[Rpt])
                    for r in range(nr):
                        op("dve", lambda e, r=r, pt=pt, hp=hp: e.tensor_tensor(
                            out=pt[:, r, :, :], in0=pt[:, r, :, :], in1=EB[:, rcs[r], 2 * hp:2 * hp + 2, :],
                            op=ALU.mult), reads=[Rpt, Rconst], writes=[Rpt])

                def pv_part(hp):
                    pt = PTa[hp % 2]
                    Rpt = RPTa[hp % 2]
                    for e_ in range(2):
                        h = 2 * hp + e_
                        bk = 4 + h // 4
                        for r in range(nr):
                            ks = slot(i - r)
                            op("pe", lambda e, bk=bk, h=h, r=r, ks=ks, e_=e_, pt=pt: e.matmul(
                                B[bk][:, (h % 4) * 65:(h % 4) * 65 + 65], pt[:, r, e_, :], vh[:, ks, h, :],
                                start=(r == 0), stop=(r == nr - 1), skip_group_check=True),
                               reads=[Rpt, Rvh[ks]], writes=[RB[bk]])

                st_part(0)
                for hp in range(4):
                    if hp + 1 < 4:
                        st_part(hp + 1)
                    pv_part(hp)
                for q in range(2):
                    bk = 4 + q
                    pv_ = B[bk][:, 0:260].rearrange("p (h d) -> p h d", h=4)
                    op("dve", lambda e, q=q, pv_=pv_: e.reciprocal(out=st[:, 8 + 4 * q:12 + 4 * q],
                                                                  in_=pv_[:, :, 64]),
                       reads=[RB[bk]], writes=[Rst2])
                    op("dve", lambda e, q=q, pv_=pv_: e.tensor_tensor(
                        out=ya[:, 256 * q:256 * q + 256].rearrange("p (h d) -> p h d", h=4),
                        in0=pv_[:, :, 0:64],
                        in1=st[:, 8 + 4 * q:12 + 4 * q].unsqueeze(2).to_broadcast([128, 4, 64]),
                        op=ALU.mult), reads=[RB[bk], Rst2], writes=[Rya])
                for c_ in range(4):
                    op("pe", lambda e, c_=c_: e.transpose(out=Tb[1][:, 512 + c_ * 128:512 + (c_ + 1) * 128],
                                                          in_=ya[:, c_ * 128:(c_ + 1) * 128], identity=identb[:]),
                       reads=[Rya, Rconst], writes=[RT[1]])
                op("act", lambda e: e.activation(out=yaT[:].rearrange("p c t -> p (c t)"), in_=Tb[1][:, 512:1024],
                                                 func=AF.Copy), reads=[RT[1]], writes=[RyaT])

                yield 'Q1END'
                for h in range(4):
                    op("pe", lambda e, h=h: e.transpose(out=Tb[1][:, h * 128:(h + 1) * 128], in_=kbT[:, h, :],
                                                        identity=identb[:]),
                       reads=[RkbT, Rconst], writes=[RT[1]])
                for h in range(4):
                    op("pe", lambda e, h=h: e.matmul(B[1][:, h * 128:(h + 1) * 128], kbT[:, h, :], qbT[:, h, :],
                                                     start=True, stop=True, skip_group_check=True),
                       reads=[RkbT, RqbT], writes=[RB[1]])
                op("act", lambda e: e.activation(out=st[:, 16:20], in_=gsm[:, 4:8], func=AF.Exp, scale=-1.0),
                   reads=[Rgsm], writes=[Rst3])
                op("act", lambda e: e.activation(out=st[:, 20:24], in_=st[:, 16:20], func=AF.Ln, bias=cst[:, 1:2]),
                   reads=[Rst3, Rconst], writes=[Rst3])
                for q in range(4):
                    op("pe", lambda e, q=q: e.matmul(B[0][:, 4 * q:4 * q + 4], consts[:, 1 + q, :], st[:, 20:24],
                                                     start=(q == 0), stop=(q == 3), skip_group_check=True),
                       reads=[Rconst, Rst3], writes=[RB[0]])
                op("dve", lambda e: e.tensor_tensor(out=st[:, 24:28], in0=gsm[:, 0:4], in1=B[0][:, 0:4], op=ALU.add),
                   reads=[Rgsm, RB[0]], writes=[Rst3])
                op("dve", lambda e: e.tensor_tensor(out=st[:, 28:32], in0=gsm[:, 0:4], in1=B[0][:, 4:8],
                                                    op=ALU.subtract), reads=[Rgsm, RB[0]], writes=[Rst3])
                op("act", lambda e: e.activation(out=st[:, 32:36], in_=B[0][:, 0:4], func=AF.Exp, scale=-1.0),
                   reads=[RB[0]], writes=[Rst3])
                op("act", lambda e: e.activation(out=st[:, 36:44], in_=st[:, 24:32], func=AF.Exp, bias=cst[:, 2:3]),
                   reads=[Rst3, Rconst], writes=[Rst3])
                op("act", lambda e: e.activation(out=st[:, 44:52], in_=B[0][:, 8:16], func=AF.Exp, scale=-1.0),
                   reads=[RB[0]], writes=[Rst3])
                yield 'q2'
                for h in range(4):
                    for c_ in range(2):
                        ps_ = slice(64 * c_, 64 * c_ + 64)
                        op("dve", lambda e: e.tensor_scalar(out=kwz[ps_, c_, h, :],
                                                            in0=Tb[1][ps_, h * 128:(h + 1) * 128],
                                                            scalar1=st[ps_, 40 + h:41 + h], scalar2=None,
                                                            op0=ALU.mult),
                           reads=[RT[1], Rst3], writes=[Rkw])
                yield 'q2'
                for h in range(4):
                    op("dve", lambda e, h=h: e.scalar_tensor_tensor(
                        out=PTm[:, h, :], in0=B[1][:, h * 128:(h + 1) * 128], scalar=st[:, 36 + h:37 + h],
                        in1=consts[:, 1, :], op0=ALU.mult, op1=ALU.mult),
                       reads=[RB[1], Rst3, Rconst], writes=[RPTm])
                yield 'q2'
                for h in range(4):
                    bk = 2 + h // 2
                    reg = B[bk][:, (h % 2) * 129:(h % 2) * 129 + 129]
                    op("pe", lambda e, reg=reg, h=h: e.matmul(reg, PTm[:, h, :], vba[:, h, :],
                                                              start=(h % 2 == 0), stop=False,
                                                              skip_group_check=True),
                       reads=[RPTm, Rvba], writes=[RB[bk]])
                for h in range(4):
                    bk = 2 + h // 2
                    reg = B[bk][:, (h % 2) * 129:(h % 2) * 129 + 129]
                    op("pe", lambda e, reg=reg, h=h, cp=cpar: e.matmul(reg, zq[:, h, 0, :], Cbf[cp][:, h, :],
                                                                       start=False, stop=False,
                                                                       skip_group_check=True),
                       reads=[Rzq, RCbf[cpar]], writes=[RB[bk]])
                yield 'q2'
                for c_ in range(2):
                    for h in range(4):
                        bk = 4 + h // 2
                        reg = B[bk][:, (h % 2) * 129:(h % 2) * 129 + 129]
                        op("pe", lambda e, reg=reg, h=h, c_=c_: e.matmul(
                            reg, kwz[:, c_, h, :], vba[:, h, :],
                            start=(h % 2 == 0), stop=True, skip_group_check=True),
                           reads=[Rkw, Rvba], writes=[RB[bk]])
                    for h in range(4):
                        bk = 4 + h // 2
                        reg = B[bk][:, (h % 2) * 129:(h % 2) * 129 + 129]
                        op("dve", lambda e, reg=reg, h=h, c_=c_: e.scalar_tensor_tensor(
                            out=Cst[:, h, :], in0=Cst[:, h, :], scalar=st[:, 44 + 4 * c_ + h:45 + 4 * c_ + h],
                            in1=reg, op0=ALU.mult, op1=ALU.add),
                           reads=[RC, Rst3, RB[bk]], writes=[RC])
                    npar = 1 - cpar
                    op("pool", lambda e, npar=npar: e.tensor_copy(out=Cbf[npar][:], in_=Cst[:]),
                       reads=[RC], writes=[RCbf[npar]])
                    cpar = npar
                    if c_ == 0:
                        for h in range(4):
                            bk = 2 + h // 2
                            reg = B[bk][:, (h % 2) * 129:(h % 2) * 129 + 129]
                            op("pe", lambda e, reg=reg, h=h, cp=cpar: e.matmul(
                                reg, zq[:, h, 1, :], Cbf[cp][:, h, :], start=False, stop=True,
                                skip_group_check=True),
                               reads=[Rzq, RCbf[cpar]], writes=[RB[bk]])
                    yield 'q2'
                for q in range(2):
                    bk = 2 + q
                    nv = B[bk][:, 0:258].rearrange("p (h d) -> p h d", h=2)
                    op("dve", lambda e, q=q, nv=nv: e.tensor_tensor(
                        out=st[:, 52 + 2 * q:54 + 2 * q], in0=nv[:, :, 128], in1=st[:, 32 + 2 * q:34 + 2 * q],
                        op=ALU.mult), reads=[RB[bk], Rst3], writes=[Rst3])
                op("dve", lambda e: e.tensor_scalar(out=st[:, 4:8], in0=st[:, 52:56], scalar1=-1.0, scalar2=None,
                                                    op0=ALU.mult), reads=[Rst3], writes=[Rst3])
                op("dve", lambda e: e.scalar_tensor_tensor(out=st[:, 52:56], in0=st[:, 52:56], scalar=1.0,
                                                           in1=st[:, 4:8], op0=ALU.max, op1=ALU.max),
                   reads=[Rst3], writes=[Rst3])
                op("dve", lambda e: e.reciprocal(out=st[:, 52:56], in_=st[:, 52:56]), reads=[Rst3], writes=[Rst3])
                op("dve", lambda e: e.tensor_tensor(out=st[:, 56:60], in0=st[:, 32:36], in1=st[:, 52:56],
                                                    op=ALU.mult), reads=[Rst3], writes=[Rst3])
                for q in range(2):
                    bk = 2 + q
                    nv = B[bk][:, 0:258].rearrange("p (h d) -> p h d", h=2)
                    op("dve", lambda e, q=q, nv=nv: e.tensor_tensor(
                        out=hb[:, 2 * q:2 * q + 2, :], in0=nv[:, :, 0:128],
                        in1=st[:, 56 + 2 * q:58 + 2 * q].unsqueeze(2).to_broadcast([128, 2, 128]),
                        op=ALU.mult), reads=[RB[bk], Rst3], writes=[Rhb])
                tmpv = tmp[:, 0:512].rearrange("p (h d) -> p h d", h=4)
                op("dve", lambda e: e.tensor_tensor(out=tmpv, in0=hb, in1=hb, op=ALU.mult),
                   reads=[Rhb], writes=[Rtmp])
                op("dve", lambda e: e.tensor_reduce(out=st[:, 60:64], in_=tmpv, axis=AX.X, op=ALU.add),
                   reads=[Rtmp], writes=[Rst3])
                op("pool", lambda e: e.tensor_scalar(out=st[:, 4:8], in0=st[:, 60:64], scalar1=1.0 / 128, scalar2=EPS,
                                                     op0=ALU.mult, op1=ALU.add), reads=[Rst3], writes=[Rst3])
                op("pool", lambda e: e.tensor_tensor(out=st[:, 60:64], in0=st[:, 4:8], in1=nh4[:], op=ALU.pow),
                   reads=[Rst3, Rconst], writes=[Rst3])
                op("dve", lambda e: e.tensor_tensor(
                    out=tmpv, in0=hb, in1=st[:, 60:64].unsqueeze(2).to_broadcast([128, 4, 128]), op=ALU.mult),
                   reads=[Rhb, Rst3], writes=[Rtmp])
                op("dve", lambda e: e.tensor_tensor(out=yb[:], in0=tmp[:, 0:512], in1=gsb[:], op=ALU.mult),
                   reads=[Rtmp, Rgsb], writes=[Ryb])
                for c_ in range(4):
                    op("pe", lambda e, c_=c_: e.transpose(out=Tb[1][:, 512 + c_ * 128:512 + (c_ + 1) * 128],
                                                          in_=yb[:, c_ * 128:(c_ + 1) * 128], identity=identb[:]),
                       reads=[Ryb, Rconst], writes=[RT[1]])
                op("act", lambda e: e.activation(out=ybT[:].rearrange("p c t -> p (c t)"), in_=Tb[1][:, 512:1024],
                                                 func=AF.Copy), reads=[RT[1]], writes=[RybT])

                cpar_box[0] = cpar
                yield 'Q2END'
                for q in range(2):
                    for n in range(4):
                        cn = (4 * q + n) * 128
                        for kc in range(4):
                            op("pe", lambda e, q=q, n=n, kc=kc, cn=cn: e.matmul(
                                B[q][:, n * 128:(n + 1) * 128], wba[:, kc, cn:cn + 128], yaT[:, kc, :],
                                start=(kc == 0), stop=(kc == 3), skip_group_check=True),
                               reads=[Rw, RyaT], writes=[RB[q]])
                    for n in range(4):
                        cn = (4 * q + n) * 128
                        for kc in range(4):
                            op("pe", lambda e, q=q, n=n, kc=kc, cn=cn: e.matmul(
                                B[2 + q][:, n * 128:(n + 1) * 128], wbb[:, kc, cn:cn + 128], ybT[:, kc, :],
                                start=(kc == 0), stop=(kc == 3), skip_group_check=True),
                               reads=[Rw, RybT], writes=[RB[2 + q]])
                    sgav = sga[:, 4 * q:4 * q + 4, :].rearrange("p m t -> p (m t)")
                    sgbv = sgb[:, 4 * q:4 * q + 4, :].rearrange("p m t -> p (m t)")
                    op("dve", lambda e, q=q, sgav=sgav: e.tensor_tensor(out=tmp[:, 0:512], in0=B[q][:], in1=sgav,
                                                                        op=ALU.mult),
                       reads=[RB[q], Rsga], writes=[Rtmp])
                    op("dve", lambda e, q=q, sgbv=sgbv: e.tensor_tensor(out=tmp[:, 512:1024], in0=B[2 + q][:],
                                                                        in1=sgbv, op=ALU.mult),
                       reads=[RB[2 + q], Rsgb], writes=[Rtmp])
                    op("dve", lambda e, q=q: e.tensor_tensor(
                        out=yT[:, 4 * q:4 * q + 4, :].rearrange("p m t -> p (m t)"), in0=tmp[:, 0:512],
                        in1=tmp[:, 512:1024], op=ALU.add), reads=[Rtmp], writes=[RyT])
                    yield 'q3'
                yield 'Q3END'
                for hf in range(2):
                    bk = 4 + hf
                    for k in range(8):
                        op("pe", lambda e, bk=bk, k=k, hf=hf: e.matmul(
                            B[bk][:], yT[:, k, :], wout[:, k, hf * 512:(hf + 1) * 512],
                            start=(k == 0), stop=(k == 7), skip_group_check=True),
                           reads=[RyT, Rw], writes=[RB[bk]])
                    op("dve", lambda e, bk=bk, hf=hf: e.tensor_tensor(
                        out=tmp[:, hf * 512:(hf + 1) * 512], in0=B[bk][:], in1=gt1[:, hf * 512:(hf + 1) * 512],
                        op=ALU.mult), reads=[RB[bk], RmodG], writes=[Rtmp])
                    yield 'q4'
                op("pool", lambda e, j=j: e.tensor_tensor(out=xb[j][:], in0=xb[j][:], in1=tmp[:], op=ALU.add),
                   reads=[Rtmp, Rxb[j]], writes=[Rxb[j]])
                dst = x1d if do_b else out
                S_.dma("sp", cxs[j], dst[row0:row0 + 128, :], xb[j][:], reads=[Rxb[j]])
                yield 'q4'

            tilesA = [(b, i) for b in range(NB) for i in range(NT)]
            for _ in sa1(*tilesA[0]):
                pass
            for n_, (b, i) in enumerate(tilesA):
                g2_ = sa2(b, i)
                g1_ = sa1(*tilesA[n_ + 1]) if n_ + 1 < len(tilesA) else iter(())
                while next(g2_) != 'Q1END':
                    pass
                for end2, end1 in (('Q2END', 'P1END'), ('Q3END', 'P2END')):
                    d2 = d1 = False
                    while not (d2 and d1):
                        if not d2:
                            d2 = (next(g2_) == end2)
                        if not d1:
                            d1 = (next(g1_, end1) == end1)
                d2 = d1 = False
                while not (d2 and d1):
                    if not d2:
                        d2 = (next(g2_, None) is None)
                    if not d1:
                        d1 = (next(g1_, None) is None)
            S_.barrier(bres)
            S_.emit()
        if do_b:
          with ExitStack() as pb:
            totb = [0]

            def sb(name, shape, dt=F32):
                n = int(np.prod(shape[1:])) * (4 if dt in (F32, U32, I32) else 2)
                totb[0] += n
                return pb.enter_context(nc.sbuf_tensor("b_" + name, list(shape), dt))
            NUV = 18
            ND = 4
            wpq = sb("wpq", [128, 8, 2048], BF16)
            keys = sb("keys", [128, 16, 128], BF16)
            identf = sb("identf", [128, 128])
            identb = sb("identb", [128, 128], BF16)
            iota = sb("iota", [128, 8, 16, 16])
            A2 = sb("A2", [128, D]); sh2 = sb("sh2", [128, D]); gt2 = sb("gt2", [128, D]); fgb = sb("fgb", [128, D])
            cst = sb("cst", [128, 4])
            xb = [sb("xb%d" % i, [128, D]) for i in range(3)]
            tmp = sb("tmp", [128, D])
            h2bf = [sb("h2bf%d" % i, [128, D], BF16) for i in range(2)]
            tmp2 = sb("tmp2", [128, D])
            prod = [sb("prod%d" % i, [128, D], BF16) for i in range(3)]
            st2 = sb("st2", [128, 16])
            h2T = sb("h2T", [128, 8, 128], BF16)
            qTp = sb("qTp", [128, 16, 128], BF16)
            sc = sb("sc", [128, 16, 128])
            scr = sb("scr", [128, 256])
            stop = sb("stop", [128, 16, 16])
            sidx = sb("sidx", [128, 16, 16], U32)
            sidxf = sb("sidxf", [128, 16, 16])
            cand = sb("cand", [128, 8, 16, 16])
            best = sb("best", [128, 8, 16])
            ci = sb("ci", [128, 8, 16], U32)
            hi = sb("hi", [128, 8, 16], U32)
            lo = sb("lo", [128, 8, 16], U32)
            hif = sb("hif", [128, 8, 16])
            lof = sb("lof", [128, 8, 16])
            oh = cand
            i1 = sb("i1", [128, 8, 16])
            i2 = sb("i2", [128, 8, 16])
            ef = sb("ef", [128, 8, 16])
            eidx = [sb("eidx%d" % i, [128, 128], U32) for i in range(2)]
            ge = sb("ge", [128, 8, 16])
            gate = [sb("gate%d" % i, [128, 8, 16]) for i in range(2)]
            gs = sb("gs", [128, 16])
            actv = sb("actv", [128, 128])
            gl = sb("gl", [128, 128])
            wv = sb("wv", [128, 128])
            UV = [sb("UV%d" % i, [128, 2 * D], BF16) for i in range(NUV)]
            dg = [sb("dg%d" % i, [128, 128], BF16) for i in range(ND)]
            st = sb("st", [128, 16])
            print("phaseB sbuf bytes/partition:", totb[0])
            Rw = Res("b_w"); Rconst = Res("b_const"); Rmod1 = Res("b_mod1"); Rmod2 = Res("b_mod2")
            Rxb = [Res("b_xb0"), Res("b_xb1"), Res("b_xb2")]
            cxb = [S_.chan("b_xb0"), S_.chan("b_xb1"), S_.chan("b_xb2")]
            cxs = [S_.chan("b_xs0"), S_.chan("b_xs1"), S_.chan("b_xs2")]
            cm = S_.chan("b_misc")
            cm2 = S_.chan("b_misc2")
            Rtmp = Res("b_tmp"); Rtmp2 = Res("b_tmp2"); Rst2 = Res("b_st2"); Rprod = [Res("prod0"), Res("prod1"), Res("prod2")]; Rh2bf = [Res("h2bf0"), Res("h2bf1")]; Rh2T = Res("h2T"); RqTp = Res("qTp")
            Rsc = Res("sc"); Rscr2 = [Res("scr0"), Res("scr1")]; Rstop = [Res("stop%d" % i) for i in range(16)]; Rsidx = [Res("sidx%d" % i) for i in range(16)]; Rsidxf = Res("sidxf")
            Rcand = Res("cand"); Rbest = [Res("best%d" % i) for i in range(8)]; Rci = [Res("ci%d" % i) for i in range(8)]; Rhl = Res("hl"); Roh = Rcand
            Ri12 = Res("i12"); Reidx = [Res("eidx0"), Res("eidx1")]; Rge = Res("ge"); Rgate = [Res("gate0"), Res("gate1")]; Rgs = Res("gs")
            Ractv = [Res("actv%d" % i) for i in range(128)]; Rgl = [Res("gl%d" % i) for i in range(4)]; Rwv = [Res("wv%d" % i) for i in range(4)]
            RUV = [Res("UV%d" % i) for i in range(NUV)]
            cUV = [S_.chan("UV%d" % i) for i in range(NUV)]
            cUVs = [S_.chan("UVs%d" % i) for i in range(NUV)]
            Ruvd = [Res("uvd%d" % i) for i in range(NUV)]
            Rdg = [Res("dg%d" % i) for i in range(ND)]
            Rst = Res("b_st")

            S_.dma("sp", cm, identf[:], consts_d[:, 0, :], writes=[Rconst])
            S_.dma("sp", cm, iota[:].rearrange("p a b c -> p (a b c)"), iota_d[:, :], writes=[Rconst])
            S_.dma("sp", cm, fgb[:], fg[0:1, :].partition_broadcast(128), writes=[Rconst])
            op("dve", lambda e: e.tensor_copy(out=identb[:], in_=identf[:]), reads=[Rconst], writes=[Rconst])
            op("pool", lambda e: e.memset(cst[:, 0:1], EPS), writes=[Rconst])
            ci_ = [0]

            def load_cast_b(dst, src, ncols):
                jx = ci_[0] % 3
                eng = ("dve", "act", "pool")[ci_[0] % 3]
                ci_[0] += 1
                S_.dma("sp", cxb[jx], xb[jx][:, 0:ncols], src, writes=[Rxb[jx]])
                if eng == "act":
                    op("act", lambda e: e.activation(out=dst, in_=xb[jx][:, 0:ncols], func=AF.Copy),
                       reads=[Rxb[jx]], writes=[Rw])
                else:
                    op(eng, lambda e: e.tensor_copy(out=dst, in_=xb[jx][:, 0:ncols]), reads=[Rxb[jx]], writes=[Rw])
            for k in range(8):
                for c0_ in range(0, 2048, 1024):
                    load_cast_b(wpq[:, k, c0_:c0_ + 1024], wpq_d[k * 128:(k + 1) * 128, c0_:c0_ + 1024], 1024)
            for q in range(2):
                load_cast_b(keys[:, 8 * q:8 * q + 8, :].rearrange("p g n -> p (g n)"),
                            keysT[:, 8 * q:8 * q + 8, :].rearrange("p g n -> p (g n)"), 1024)
            def stage1(b, i):
                g = b * NT + i
                j = g % 2
                jx = g % 3
                row0 = g * 128
                if i == 0:
                    S_.dma("sp", cm, A2[:], g2[0:1, :].partition_broadcast(128), writes=[Rmod1])
                    S_.dma("sp", cm, tmp[:], modrow[b:b + 1, 4 * D:5 * D].partition_broadcast(128), writes=[Rtmp])
                    S_.dma("sp", cm, sh2[:], modrow[b:b + 1, 3 * D:4 * D].partition_broadcast(128), writes=[Rmod1])
                    op("dve", lambda e: e.scalar_tensor_tensor(out=A2[:], in0=tmp[:], scalar=1.0, in1=A2[:],
                                                               op0=ALU.add, op1=ALU.mult),
                       reads=[Rtmp, Rmod1], writes=[Rmod1])
                S_.dma("sp", cxb[jx], xb[jx][:], x1d[row0:row0 + 128, :], writes=[Rxb[jx]])
                op("act", lambda e: e.activation(out=tmp[:], in_=xb[jx][:], func=AF.Square, scale=1.0 / 32,
                                                 accum_out=st[:, 0:1]), reads=[Rxb[jx]], writes=[Rtmp, Rst])
                op("act", lambda e: e.activation(out=st[:, 1:2], in_=st[:, 0:1], func=AF.Sqrt, bias=cst[:, 0:1]),
                   reads=[Rst, Rconst], writes=[Rst])
                op("dve", lambda e: e.reciprocal(out=st[:, 2:3], in_=st[:, 1:2]), reads=[Rst], writes=[Rst])
                op("dve", lambda e: e.scalar_tensor_tensor(out=tmp[:], in0=xb[jx][:], scalar=st[:, 2:3],
                                                           in1=A2[:], op0=ALU.mult, op1=ALU.mult),
                   reads=[Rxb[jx], Rst, Rmod1], writes=[Rtmp])
                op("dve", lambda e: e.tensor_tensor(out=h2bf[j][:], in0=tmp[:], in1=sh2[:], op=ALU.add),
                   reads=[Rtmp, Rmod1], writes=[Rh2bf[j]])
                for k in range(8):
                    op("pe", lambda e: e.transpose(out=Tb[0][:, k * 128:(k + 1) * 128],
                                                   in_=h2bf[j][:, k * 128:(k + 1) * 128], identity=identb[:]),
                       reads=[Rh2bf[j], Rconst], writes=[RT[0]])
                op("act", lambda e: e.activation(out=h2T[:].rearrange("p k t -> p (k t)"), in_=Tb[0][:],
                                                 func=AF.Copy), reads=[RT[0]], writes=[Rh2T])
                yield
                for q in range(4):
                    for m in range(4):
                        gq = 4 * q + m
                        for k in range(8):
                            op("pe", lambda e: e.matmul(B[q % 2][:, m * 128:(m + 1) * 128],
                                                        wpq[:, k, gq * 128:(gq + 1) * 128], h2T[:, k, :],
                                                        start=(k == 0), stop=(k == 7), skip_group_check=True),
                               reads=[Rw, Rh2T], writes=[RB[q % 2]])
                    op("act", lambda e: e.activation(out=qTp[:, 4 * q:4 * q + 4, :].rearrange("p g t -> p (g t)"),
                                                     in_=B[q % 2][:], func=AF.Copy), reads=[RB[q % 2]], writes=[RqTp])
                yield
                for q in range(4):
                    for m in range(4):
                        gq = 4 * q + m
                        op("pe", lambda e: e.matmul(B[q % 2][:, m * 128:(m + 1) * 128], qTp[:, gq, :], keys[:, gq, :],
                                                    start=True, stop=True, skip_group_check=True),
                           reads=[RqTp, Rw], writes=[RB[q % 2]])
                    op("act", lambda e: e.activation(out=sc[:, 4 * q:4 * q + 4, :].rearrange("p g n -> p (g n)"),
                                                     in_=B[q % 2][:], func=AF.Copy), reads=[RB[q % 2]], writes=[Rsc])
                yield
                for gq in range(16):
                    sq = scr[:, 128 * (gq % 2):128 * (gq % 2) + 128]
                    Rsq = Rscr2[gq % 2]
                    op("dve", lambda e: e.max(out=stop[:, gq, 0:8], in_=sc[:, gq, :]), reads=[Rsc], writes=[Rstop[gq]])
                    yield
                    op("dve", lambda e: e.max_index(out=sidx[:, gq, 0:8], in_max=stop[:, gq, 0:8],
                                                    in_values=sc[:, gq, :]), reads=[Rsc, Rstop[gq]], writes=[Rsidx[gq]])
                    yield
                    op("dve", lambda e: e.match_replace(out=sq, in_to_replace=stop[:, gq, 0:8],
                                                        in_values=sc[:, gq, :], imm_value=NEG),
                       reads=[Rsc, Rstop[gq]], writes=[Rsq])
                    yield
                    op("dve", lambda e: e.max(out=stop[:, gq, 8:16], in_=sq), reads=[Rsq],
                       writes=[Rstop[gq]])
                    yield
                    op("dve", lambda e: e.max_index(out=sidx[:, gq, 8:16], in_max=stop[:, gq, 8:16],
                                                    in_values=sq), reads=[Rsq, Rstop[gq]], writes=[Rsidx[gq]])
                    yield
                stv = stop[:].rearrange("p (h two) k -> p h two k", two=2)
                op("dve", lambda e: e.tensor_tensor(
                    out=cand[:], in0=stv[:, :, 0, :].unsqueeze(3).to_broadcast([128, 8, 16, 16]),
                    in1=stv[:, :, 1, :].unsqueeze(2).to_broadcast([128, 8, 16, 16]), op=ALU.add),
                   reads=Rstop, writes=[Rcand])
                op("act", lambda e: e.activation(out=sidxf[:], in_=sidx[:], func=AF.Copy), reads=Rsidx,
                   writes=[Rsidxf])
                yield
                for h in range(8):
                    cv = cand[:, h, :, :].rearrange("p a b -> p (a b)")
                    op("dve", lambda e: e.max(out=best[:, h, 0:8], in_=cv), reads=[Rcand], writes=[Rbest[h]])
                    yield
                    op("dve", lambda e: e.max_index(out=ci[:, h, 0:8], in_max=best[:, h, 0:8], in_values=cv),
                       reads=[Rcand, Rbest[h]], writes=[Rci[h]])
                    yield
                    op("dve", lambda e: e.match_replace(out=scr[:], in_to_replace=best[:, h, 0:8], in_values=cv,
                                                        imm_value=NEG), reads=[Rcand, Rbest[h]], writes=Rscr2)
                    yield
                    op("dve", lambda e: e.max(out=best[:, h, 8:16], in_=scr[:]), reads=Rscr2, writes=[Rbest[h]])
                    yield
                    op("dve", lambda e: e.max_index(out=ci[:, h, 8:16], in_max=best[:, h, 8:16], in_values=scr[:]),
                       reads=Rscr2 + [Rbest[h]], writes=[Rci[h]])
                    yield
                op("dve", lambda e: e.tensor_tensor(out=ge[:], in0=best[:],
                                                    in1=best[:, :, 0:1].to_broadcast([128, 8, 16]),
                                                    op=ALU.subtract), reads=Rbest, writes=[Rge])
                op("act", lambda e: e.activation(out=ge[:], in_=ge[:], func=AF.Exp), reads=[Rge], writes=[Rge])
                op("dve", lambda e: e.tensor_reduce(out=gs[:, 0:8], in_=ge[:], axis=AX.X, op=ALU.add),
                   reads=[Rge], writes=[Rgs])
                op("dve", lambda e: e.reciprocal(out=gs[:, 8:16], in_=gs[:, 0:8]), reads=[Rgs], writes=[Rgs])
                op("dve", lambda e: e.tensor_tensor(out=gate[j][:], in0=ge[:],
                                                    in1=gs[:, 8:16].unsqueeze(2).to_broadcast([128, 8, 16]),
                                                    op=ALU.mult), reads=[Rge, Rgs], writes=[Rgate[j]])
                op("dve", lambda e: e.tensor_single_scalar(out=hi[:], in_=ci[:], scalar=4,
                                                           op=ALU.logical_shift_right), reads=Rci, writes=[Rhl])
                op("dve", lambda e: e.tensor_single_scalar(out=lo[:], in_=ci[:], scalar=15, op=ALU.bitwise_and),
                   reads=Rci, writes=[Rhl])
                op("act", lambda e: e.activation(out=hif[:], in_=hi[:], func=AF.Copy), reads=[Rhl], writes=[Rhl])
                op("act", lambda e: e.activation(out=lof[:], in_=lo[:], func=AF.Copy), reads=[Rhl], writes=[Rhl])
                sxv = sidxf[:].rearrange("p (h two) k -> p h two k", two=2)
                for p_, (src, dsti) in enumerate(((hif, i1), (lof, i2))):
                    op("dve", lambda e: e.tensor_tensor(
                        out=oh[:], in0=src[:].unsqueeze(3).to_broadcast([128, 8, 16, 16]), in1=iota[:],
                        op=ALU.is_equal), reads=[Rhl, Rconst], writes=[Roh])
                    op("dve", lambda e: e.tensor_tensor(
                        out=oh[:], in0=oh[:], in1=sxv[:, :, p_, :].unsqueeze(2).to_broadcast([128, 8, 16, 16]),
                        op=ALU.mult), reads=[Roh, Rsidxf], writes=[Roh])
                    op("dve", lambda e: e.tensor_reduce(out=dsti[:], in_=oh[:], axis=AX.X, op=ALU.add),
                       reads=[Roh], writes=[Ri12])
                op("dve", lambda e: e.scalar_tensor_tensor(out=ef[:], in0=i1[:], scalar=128.0, in1=i2[:],
                                                           op0=ALU.mult, op1=ALU.add), reads=[Ri12], writes=[Ri12])
                op("dve", lambda e: e.tensor_copy(out=eidx[j][:].rearrange("p (h k) -> p h k", h=8), in_=ef[:]),
                   reads=[Ri12], writes=[Reidx[j]])
                yield

            def stage2(b, i, nxt, prev_final):
                g = b * NT + i
                j = g % 2
                jx = g % 3
                row0 = g * 128
                vb0 = 4 if g % 2 == 0 else 2
                GS = 4

                def finish(q):
                    c0_ = q * GS
                    hq = c0_ // 16
                    op("act", lambda e: e.activation(out=gl[:, c0_:c0_ + GS], in_=actv[:, c0_:c0_ + GS], func=AF.Gelu),
                       reads=Ractv[c0_:c0_ + GS], writes=[Rgl[q % 4]])
                    op("dve", lambda e: e.tensor_tensor(out=wv[:, c0_:c0_ + GS], in0=gl[:, c0_:c0_ + GS],
                                                        in1=gate[j][:, hq, c0_ - 16 * hq:c0_ - 16 * hq + GS],
                                                        op=ALU.mult), reads=[Rgl[q % 4], Rgate[j]], writes=[Rwv[q % 4]])
                    for jj in range(c0_, c0_ + GS):
                        su = (g * 128 + jj) % NUV
                        sd = jj % ND
                        op("act", lambda e: e.activation(out=dg[sd][:], in_=identb[:], func=AF.Copy,
                                                         scale=wv[:, jj:jj + 1]),
                           reads=[Rconst, Rwv[q % 4]], writes=[Rdg[sd]])
                        for hf in range(2):
                            op("pe", lambda e: e.matmul(B[vb0 + hf][:], dg[sd][:],
                                                        UV[su][:, D + hf * 512:D + (hf + 1) * 512],
                                                        start=(jj == 0), stop=(jj == 127), skip_group_check=True),
                               reads=[Rdg[sd], RUV[su]], writes=[RB[vb0 + hf]])

                NG = 128 // GS
                for q in range(NG):
                    for jj in range(q * GS, (q + 1) * GS):
                        su = (g * 128 + jj) % NUV
                        pp = (jj // 3) % 3
                        op("pool", lambda e: e.indirect_dma_start(
                            out=UV[su][:], out_offset=None, in_=uvd[:, :],
                            in_offset=bass.IndirectOffsetOnAxis(ap=eidx[j][:, jj:jj + 1], axis=0)),
                           reads=[Reidx[j]], writes=[RUV[su]], chan=cUV[su])
                        if jj % 2 == 0:
                            op("dve", lambda e: e.tensor_tensor(out=prod[pp][:], in0=UV[su][:, 0:D], in1=h2bf[j][:],
                                                                op=ALU.mult),
                               reads=[RUV[su], Rh2bf[j]], writes=[Rprod[pp]])
                            op("act", lambda e: e.activation(out=prod[pp][:], in_=prod[pp][:], func=AF.Copy,
                                                             accum_out=actv[:, jj:jj + 1]),
                               reads=[Rprod[pp]], writes=[Rprod[pp], Ractv[jj]])
                        else:
                            op("dve", lambda e: e.scalar_tensor_tensor(
                                out=UV[su][:, 0:D], in0=UV[su][:, 0:D], scalar=1.0, in1=h2bf[j][:], op0=ALU.mult,
                                op1=ALU.mult, accum_out=actv[:, jj:jj + 1]),
                               reads=[RUV[su], Rh2bf[j]], writes=[RUV[su], Ractv[jj]])
                    if q >= 2:
                        finish(q - 2)
                    if q == 2:
                        if prev_final is not None:
                            prev_final()
                        if i == 0:
                            S_.dma("sp", cm2, gt2[:], modrow[b:b + 1, 5 * D:6 * D].partition_broadcast(128),
                                   writes=[Rmod2])
                    if nxt is not None:
                        for _ in range(2 * GS):
                            next(nxt, None)
                finish(NG - 2)
                finish(NG - 1)
                if nxt is not None:
                    for _ in nxt:
                        pass

                def final():
                    for hf in range(2):
                        op("dve", lambda e: e.tensor_tensor(out=tmp2[:, hf * 512:(hf + 1) * 512], in0=B[vb0 + hf][:],
                                                            in1=gt2[:, hf * 512:(hf + 1) * 512], op=ALU.mult),
                           reads=[RB[vb0 + hf], Rmod2], writes=[Rtmp2])
                    op("dve", lambda e: e.tensor_tensor(out=xb[jx][:], in0=xb[jx][:], in1=tmp2[:], op=ALU.add),
                       reads=[Rtmp2, Rxb[jx]], writes=[Rxb[jx]])
                    op("act", lambda e: e.activation(out=tmp2[:], in_=xb[jx][:], func=AF.Square, scale=1.0 / 32,
                                                     accum_out=st2[:, 4:5]), reads=[Rxb[jx]], writes=[Rtmp2, Rst2])
                    op("act", lambda e: e.activation(out=st2[:, 5:6], in_=st2[:, 4:5], func=AF.Sqrt,
                                                     bias=cst[:, 0:1]),
                       reads=[Rst2, Rconst], writes=[Rst2])
                    op("dve", lambda e: e.reciprocal(out=st2[:, 6:7], in_=st2[:, 5:6]), reads=[Rst2], writes=[Rst2])
                    op("dve", lambda e: e.scalar_tensor_tensor(out=xb[jx][:], in0=xb[jx][:], scalar=st2[:, 6:7],
                                                               in1=fgb[:], op0=ALU.mult, op1=ALU.mult),
                       reads=[Rxb[jx], Rst2, Rconst], writes=[Rxb[jx]])
                    S_.dma("sp", cxs[jx], out[row0:row0 + 128, :], xb[jx][:], reads=[Rxb[jx]])
                return final

            tiles = [(b, i) for b in range(NB) for i in range(NT)]
            for _ in stage1(*tiles[0]):
                pass
            pf = None
            for n_, (b, i) in enumerate(tiles):
                nxt = stage1(*tiles[n_ + 1]) if n_ + 1 < len(tiles) else None
                pf = stage2(b, i, nxt, pf)
            pf()
            S_.barrier(bres)
            S_.emit()
    return nc


def _host_consts():
    c = np.zeros((128, 6, 128), np.float32)
    s = np.arange(128)[:, None]
    l = np.arange(128)[None, :]
    same = (s // 64) == (l // 64)
    c[:, 0, :] = np.eye(128, dtype=np.float32)
    c[:, 1, :] = (same & (s <= l)).astype(np.float32)
    c[:, 2, :] = (same & (s > l)).astype(np.float32)
    c[:, 3, :] = np.broadcast_to((s < 64), (128, 128)).astype(np.float32)
    c[:, 4, :] = np.broadcast_to((s >= 64), (128, 128)).astype(np.float32)
    return c


def prep_shared(inp):
    f = lambda a: np.ascontiguousarray(np.asarray(a, dtype=np.float32))
    rb = f(inp["rel_bias"])[0]
    k = np.arange(128)[:, None]
    l = np.arange(128)[None, :]
    eb = np.zeros((128, 4, 8, 128), np.float32)
    for rc, r in enumerate((0, 1, 2, 4)):
        idx = np.clip(128 * r + l - k, -128, 128) + 128
        eb[:, rc, :, :] = np.transpose(rb[:, idx], (1, 0, 2))
    d = {
        "w_ada": f(inp["w_ada"])[0],
        "b_ada": f(inp["b_ada"]),
        "norm1_g": f(inp["norm1_g"]),
        "w_in": f(inp["w_in"])[0],
        "cwT": f(f(inp["conv_w"])[0].T.reshape(8, 128, 4).transpose(1, 0, 2)),
        "cbT": f(f(inp["conv_b"])[0].reshape(8, 128).T),
        "bif": f(np.concatenate([f(inp["b_igate"]), f(inp["b_fgate"])], axis=1)),
        "ebsrc": eb,
        "gmn": f(inp["mlstm_norm_g"]),
        "w_branch_a": f(inp["w_branch_a"])[0],
        "w_branch_b": f(inp["w_branch_b"])[0],
        "w_out": f(inp["w_out"])[0],
        "norm2_g": f(inp["norm2_g"]),
        "w_peer_q": f(inp["w_peer_q"])[0],
        "keysT": f(f(inp["peer_sub_keys"])[0].reshape(16, 128, 128).transpose(2, 0, 1)),
        "peer_u": f(inp["peer_u"])[0],
        "peer_v": f(inp["peer_v"])[0],
        "final_g": f(inp["final_g"]).reshape(1, D),
        "consts": _host_consts(),
        "iota16": np.ascontiguousarray(np.broadcast_to((np.arange(2048) % 16).astype(np.float32), (128, 2048))),
    }
    return d


def prep_core(inp, b0, NB):
    f = lambda a: np.ascontiguousarray(np.asarray(a, dtype=np.float32))
    xs = f(inp["x"][b0:b0 + NB]).reshape(-1, D)
    c = f(inp["c"][b0:b0 + NB])
    cT = f(c.reshape(NB, 8, 128).transpose(2, 0, 1))
    return {"x": xs, "cT": cT}


_NC_CACHE = {}


def kernel(**inputs):
    n = 8
    NB = 2
    S = 4096
    key = (NB, S)
    if key not in _NC_CACHE:
        _NC_CACHE[key] = build(NB, S)
    nc = _NC_CACHE[key]
    shared = prep_shared(inputs)
    in_maps = []
    for i in range(n):
        m = dict(shared)
        m.update(prep_core(inputs, i * NB, NB))
        in_maps.append(m)
    res = run_bass_kernel_spmd(nc, in_maps, core_ids=list(range(n)))
    outs = [np.asarray(r["out"]).reshape(NB, S, D) for r in res.results]
    return np.concatenate(outs, axis=0).astype(np.float32)
```
